# Optimizing a Trainium2 kernel written in Bass

```python
import jax, jax.numpy as jnp
from jax import lax
import numpy as np

D_MODEL = 1024
BATCH = 16
SEQ = 2048
DEPTH = 2

ROPE_THETA = 10000.0
NORM_EPS = 1e-6
HEAD_NORM_EPS = 1e-5
RET_HEADS = 4
RET_HEAD_DIM = 256
RET_WIDTH = RET_HEADS * RET_HEAD_DIM
RET_CHUNK = 128
ATT_GROUPS = ((128, 1), (512, 4), (2048, 16))
ATT_N_GROUPS = 3
ATT_HEADS_PER_GROUP = 8
ATT_HEAD_DIM = 128
ATT_QKV_WIDTH = ATT_N_GROUPS * ATT_HEADS_PER_GROUP * ATT_HEAD_DIM
ATT_WIDTH = ATT_HEADS_PER_GROUP * ATT_HEAD_DIM
ATT_BLOCK = 128
CONV_WIDTH = 1024
CONV_TAPS = 31
N_BRANCHES = 3
IN_SPLITS = (RET_WIDTH, RET_WIDTH, RET_WIDTH, RET_WIDTH,
             ATT_QKV_WIDTH, ATT_QKV_WIDTH, ATT_QKV_WIDTH, ATT_WIDTH,
             2 * CONV_WIDTH, CONV_WIDTH, N_BRANCHES * D_MODEL)
IN_WIDTH = 4 * RET_WIDTH + 3 * ATT_QKV_WIDTH + ATT_WIDTH + 3 * CONV_WIDTH + N_BRANCHES * D_MODEL

kernel_name = "hybrid_retention_dilated_conv_gated"


def rms_norm(x, w):
    xf = x.astype(jnp.float32)
    y = xf * lax.rsqrt(jnp.mean(xf * xf, axis=-1, keepdims=True) + NORM_EPS)
    return (y * w).astype(x.dtype)


def layer_norm(x, w, b):
    xf = x.astype(jnp.float32)
    mu = jnp.mean(xf, axis=-1, keepdims=True)
    var = jnp.mean(jnp.square(xf - mu), axis=-1, keepdims=True)
    return ((xf - mu) * lax.rsqrt(var + HEAD_NORM_EPS) * w + b).astype(x.dtype)


def head_norm(x):
    xf = x.astype(jnp.float32)
    mu = jnp.mean(xf, axis=-1, keepdims=True)
    var = jnp.mean(jnp.square(xf - mu), axis=-1, keepdims=True)
    return ((xf - mu) * lax.rsqrt(var + HEAD_NORM_EPS)).astype(x.dtype)


def rope(x, pos):
    hd = x.shape[-1]
    inv = ROPE_THETA ** (-jnp.arange(0, hd, 2, dtype=jnp.float32) / hd)
    ang = pos.astype(jnp.float32)[:, None] * inv[None, :]
    shape = (1, ang.shape[0]) + (1,) * (x.ndim - 3) + (hd // 2,)
    cos = jnp.cos(ang).reshape(shape)
    sin = jnp.sin(ang).reshape(shape)
    x1, x2 = jnp.split(x.astype(jnp.float32), 2, axis=-1)
    return jnp.concatenate([x1 * cos - x2 * sin, x2 * cos + x1 * sin], axis=-1).astype(x.dtype)


def retention(q, k, v):
    B, S, H, hd = q.shape
    C = RET_CHUNK
    N = S // C
    dt = q.dtype
    lg = jnp.log(1.0 - 2.0 ** (-5.0 - jnp.arange(H, dtype=jnp.float32)))
    idx = jnp.arange(C, dtype=jnp.float32)
    diff = idx[:, None] - idx[None, :]
    intra_decay = jnp.where(diff[None] >= 0,
                            jnp.exp(jnp.maximum(diff, 0.0)[None] * lg[:, None, None]), 0.0)
    q_decay = jnp.exp((idx + 1.0)[:, None] * lg[None, :]).astype(dt)
    k_decay = jnp.exp((C - 1.0 - idx)[:, None] * lg[None, :]).astype(dt)
    chunk_decay = jnp.exp(C * lg).astype(dt)
    qc = q.reshape(B, N, C, H, hd)
    kc = k.reshape(B, N, C, H, hd)
    vc = v.reshape(B, N, C, H, hd)
    scores = jnp.einsum('bnihd,bnjhd->bnhij', qc, kc) * intra_decay.astype(dt)
    intra = jnp.einsum('bnhij,bnjhe->bnihe', scores, vc)

    def step(state, inp):
        qn, kn, vn = inp
        inter = jnp.einsum('bihd,bhde->bihe', qn, state) * q_decay[None, :, :, None]
        state = (state * chunk_decay[None, :, None, None]
                 + jnp.einsum('bjhd,bjhe->bhde', kn * k_decay[None, :, :, None], vn))
        return state, inter

    init = jnp.zeros((B, H, hd, hd), dt)
    _, inter = lax.scan(step, init, (jnp.moveaxis(qc, 1, 0), jnp.moveaxis(kc, 1, 0), jnp.moveaxis(vc, 1, 0)))
    inter = jnp.moveaxis(inter, 0, 1)
    return (intra + inter).reshape(B, S, H, hd)


def dilated_group(q, k, v, window, dilation):
    B, S, Hg, hd = q.shape
    blk = ATT_BLOCK
    L = S // dilation
    n_blk = -(-L // blk)
    Lp = n_blk * blk
    span = window // dilation
    qs = q.reshape(B, L, dilation, Hg, hd)
    ks = k.reshape(B, L, dilation, Hg, hd)
    vs = v.reshape(B, L, dilation, Hg, hd)
    qs = jnp.pad(qs, ((0, 0), (0, Lp - L), (0, 0), (0, 0), (0, 0)))
    ks = jnp.pad(ks, ((0, 0), (blk, Lp - L), (0, 0), (0, 0), (0, 0)))
    vs = jnp.pad(vs, ((0, 0), (blk, Lp - L), (0, 0), (0, 0), (0, 0)))
    qb = qs.reshape(B, n_blk, blk, dilation, Hg, hd)
    kb = ks.reshape(B, n_blk + 1, blk, dilation, Hg, hd)
    vb = vs.reshape(B, n_blk + 1, blk, dilation, Hg, hd)
    kk = jnp.concatenate([kb[:, :-1], kb[:, 1:]], axis=2)
    vv = jnp.concatenate([vb[:, :-1], vb[:, 1:]], axis=2)
    s = jnp.einsum('bnidhe,bnjdhe->bnidhj', qb, kk).astype(jnp.float32) * (hd ** -0.5)
    i = jnp.arange(blk)[:, None]
    j = jnp.arange(2 * blk)[None, :]
    dist = i - j + blk
    key_l = jnp.arange(n_blk)[:, None, None] * blk + j[None] - blk
    valid = (dist[None] >= 0) & (dist[None] <= span) & (key_l >= 0)
    s = jnp.where(valid[None, :, :, None, None, :], s, -jnp.inf)
    m = jnp.max(s, axis=-1, keepdims=True)
    p = jnp.exp(s - m)
    den = jnp.sum(p, axis=-1)
    o = jnp.einsum('bnidhj,bnjdhe->bnidhe', p, vv.astype(jnp.float32)) / den[..., None]
    lse = m[..., 0] + jnp.log(den)
    o = o.reshape(B, Lp, dilation, Hg, hd)[:, :L].reshape(B, S, Hg, hd)
    lse = lse.reshape(B, Lp, dilation, Hg)[:, :L].reshape(B, S, Hg)
    return o, lse


def hybrid_layer(x, pos, norm_w, w_in, b_in, ret_norm_w, ret_w_o, att_w_o,
                 conv_dw_w, conv_dw_b, conv_norm_w, conv_norm_b, conv_w_o, w_out):
    B, S, D = x.shape
    h = rms_norm(x, norm_w)
    z = h @ w_in + b_in
    split_points = [int(c) for c in np.cumsum(IN_SPLITS)[:-1]]
    rq, rk, rv, rg, aq, ak, av, ag, cu, cg, mg = jnp.split(z, split_points, axis=-1)

    rshp = (B, S, RET_HEADS, RET_HEAD_DIM)
    rq = rope(rq.reshape(rshp), pos)
    rk = rope(rk.reshape(rshp), pos) * (RET_HEAD_DIM ** -0.5)
    r = retention(rq, rk, rv.reshape(rshp))
    r = head_norm(r).reshape(B, S, RET_WIDTH) * ret_norm_w
    y_ret = (r * jax.nn.silu(rg)) @ ret_w_o

    ashp = (B, S, ATT_N_GROUPS, ATT_HEADS_PER_GROUP, ATT_HEAD_DIM)
    aq = rope(aq.reshape(ashp), pos)
    ak = rope(ak.reshape(ashp), pos)
    av = av.reshape(ashp)
    outs, lses = [], []
    for g, (window, dil) in enumerate(ATT_GROUPS):
        o, l = dilated_group(aq[:, :, g], ak[:, :, g], av[:, :, g], window, dil)
        outs.append(o)
        lses.append(l)
    wts = jax.nn.softmax(jnp.stack(lses, axis=0), axis=0)
    a = jnp.sum(wts[..., None] * jnp.stack(outs, axis=0), axis=0)
    a = a.astype(x.dtype).reshape(B, S, ATT_WIDTH)
    y_att = (a * jax.nn.silu(ag)) @ att_w_o

    c_lin, c_gate = jnp.split(cu, 2, axis=-1)
    c = c_lin * jax.nn.sigmoid(c_gate)
    c = lax.conv_general_dilated(c, conv_dw_w[:, None, :], window_strides=(1,),
                                 padding=[(CONV_TAPS - 1, 0)],
                                 dimension_numbers=('NWC', 'WIO', 'NWC'),
                                 feature_group_count=CONV_WIDTH) + conv_dw_b
    c = jax.nn.silu(layer_norm(c, conv_norm_w, conv_norm_b))
    y_conv = (c * jax.nn.silu(cg)) @ conv_w_o

    gates = jax.nn.sigmoid(mg).reshape(B, S, N_BRANCHES, D)
    merged = gates[:, :, 0] * y_ret + gates[:, :, 1] * y_att + gates[:, :, 2] * y_conv
    return x + merged @ w_out


def setup_inputs(seed: int = 0) -> dict:
    key = jax.random.key(seed)
    ks = jax.random.split(key, 14)
    nrm = jax.random.normal
    f32 = jnp.float32
    return {
        "x": nrm(ks[0], (BATCH, SEQ, D_MODEL), f32),
        "norm_w": 1.0 + 0.02 * nrm(ks[1], (DEPTH, D_MODEL), f32),
        "w_in": nrm(ks[2], (DEPTH, D_MODEL, IN_WIDTH), f32) * D_MODEL ** -0.5,
        "b_in": 0.02 * nrm(ks[3], (DEPTH, IN_WIDTH), f32),
        "ret_norm_w": 1.0 + 0.02 * nrm(ks[4], (DEPTH, RET_WIDTH), f32),
        "ret_w_o": nrm(ks[5], (DEPTH, RET_WIDTH, D_MODEL), f32) * RET_WIDTH ** -0.5,
        "att_w_o": nrm(ks[6], (DEPTH, ATT_WIDTH, D_MODEL), f32) * ATT_WIDTH ** -0.5,
        "conv_dw_w": nrm(ks[7], (DEPTH, CONV_TAPS, CONV_WIDTH), f32) * CONV_TAPS ** -0.5,
        "conv_dw_b": 0.02 * nrm(ks[8], (DEPTH, CONV_WIDTH), f32),
        "conv_norm_w": 1.0 + 0.02 * nrm(ks[9], (DEPTH, CONV_WIDTH), f32),
        "conv_norm_b": 0.02 * nrm(ks[10], (DEPTH, CONV_WIDTH), f32),
        "conv_w_o": nrm(ks[11], (DEPTH, CONV_WIDTH, D_MODEL), f32) * CONV_WIDTH ** -0.5,
        "w_out": nrm(ks[12], (DEPTH, D_MODEL, D_MODEL), f32) * D_MODEL ** -0.5,
        "final_norm_w": 1.0 + 0.02 * nrm(ks[13], (D_MODEL,), f32),
    }


def reference(x, norm_w, w_in, b_in, ret_norm_w, ret_w_o, att_w_o, conv_dw_w, conv_dw_b,
              conv_norm_w, conv_norm_b, conv_w_o, w_out, final_norm_w):
    pos = jnp.arange(x.shape[1], dtype=jnp.int32)
    for l in range(DEPTH):
        x = hybrid_layer(x, pos, norm_w[l], w_in[l], b_in[l], ret_norm_w[l], ret_w_o[l], att_w_o[l],
                         conv_dw_w[l], conv_dw_b[l], conv_norm_w[l], conv_norm_b[l], conv_w_o[l], w_out[l])
    return rms_norm(x, final_norm_w)
```

```python
import contextlib
import numpy as np
import concourse.bass as bass
import concourse.mybir as mybir
from concourse.bass_utils import run_bass_kernel_spmd

F32 = mybir.dt.float32
BF16 = mybir.dt.bfloat16
AF = mybir.ActivationFunctionType
ALU = mybir.AluOpType

ENGS = ("pe", "act", "dve", "pool", "sp")

D_MODEL = 1024
SEQ = 2048
DEPTH = 2
NCORES = 8
CONV_POOL = (0, 1, 2, 3, 4)
CONV_PIPE = True
NS = 2
THETA = 10000.0


class Sched:
    def __init__(self, nc):
        self.nc = nc
        self.streams = {e: [] for e in ENGS}
        self.res = {}
        self.dma_cnt = {}
        self.dma_names = []
        self.pending = {e: [] for e in ENGS}

    def _deps_for(self, eng, reads, writes):
        deps = []
        if self.pending[eng]:
            deps.extend(self.pending[eng])
            self.pending[eng] = []
        for r in reads:
            st = self.res.get(r)
            if st and st["w"] is not None:
                deps.append(st["w"])
        for w in writes:
            st = self.res.get(w)
            if st:
                if st["w"] is not None:
                    deps.append(st["w"])
                deps.extend(st["r"])
        return deps

    def _commit(self, token, reads, writes):
        for r in reads:
            st = self.res.setdefault(r, {"w": None, "r": []})
            st["r"].append(token)
            if len(st["r"]) > 16:
                seen = {}
                for t in st["r"]:
                    k = t[:2]
                    if k not in seen or seen[k][2] < t[2]:
                        seen[k] = t
                st["r"] = list(seen.values())
        for w in writes:
            self.res[w] = {"w": token, "r": []}

    def op(self, eng, fn, reads=(), writes=(), sig=True):
        deps = self._deps_for(eng, reads, writes)
        idx = len(self.streams[eng])
        self.streams[eng].append({"fn": fn, "deps": deps, "sig": sig, "dma": None})
        self._commit(("eng", eng, idx), reads, writes)

    def dma(self, q, fn, sem, reads=(), writes=()):
        deps = self._deps_for(q, reads, writes)
        if sem not in self.dma_cnt:
            self.dma_cnt[sem] = 0
            self.dma_names.append(sem)
        self.dma_cnt[sem] += 16
        self.streams[q].append({"fn": fn, "deps": deps, "sig": False, "dma": sem})
        self._commit(("dma", sem, self.dma_cnt[sem]), reads, writes)

    def barrier(self):
        toks = []
        for e in ENGS:
            st = self.streams[e]
            for i in range(len(st) - 1, -1, -1):
                if st[i]["dma"] is None:
                    toks.append(("eng", e, i))
                    break
        for n in self.dma_names:
            toks.append(("dma", n, self.dma_cnt[n]))
        for e in ENGS:
            self.pending[e] = list(toks)

    def emit(self, final_waits=()):
        nc = self.nc
        sigcnt = {}
        for e in ENGS:
            st = self.streams[e]
            for o in reversed(st):
                if o["dma"] is None:
                    o["sig"] = True
                    break
            c = 0
            arr = []
            for o in st:
                if o["dma"] is None and o["sig"]:
                    c += 1
                arr.append(c)
            need = [None] * len(st)
            nxt = None
            for i in range(len(st) - 1, -1, -1):
                if st[i]["dma"] is None and st[i]["sig"]:
                    nxt = arr[i]
                need[i] = nxt
            sigcnt[e] = need
        with contextlib.ExitStack() as es:
            esem = {e: es.enter_context(nc.semaphore("s_" + e)) for e in ENGS}
            dsem = {n: es.enter_context(nc.semaphore("d_" + n)) for n in self.dma_names}
            block = es.enter_context(nc.Block())

            def run(e, eng):
                waited = {}
                for o in self.streams[e]:
                    wl = {}
                    for d in o["deps"]:
                        if d[0] == "eng":
                            if d[1] == e and e == "pe":
                                continue
                            key = ("eng", d[1])
                            val = sigcnt[d[1]][d[2]]
                        else:
                            key = ("dma", d[1])
                            val = d[2]
                        if val is None or waited.get(key, 0) >= val:
                            continue
                        if wl.get(key, 0) < val:
                            wl[key] = val
                    for key, val in wl.items():
                        sem = esem[key[1]] if key[0] == "eng" else dsem[key[1]]
                        eng.wait_ge(sem, val)
                        waited[key] = val
                    ins = o["fn"](eng)
                    if o["dma"] is not None:
                        ins.then_inc(dsem[o["dma"]], 16)
                    elif o["sig"]:
                        ins.then_inc(esem[e], 1)
                if e == "sp":
                    for n in final_waits:
                        eng.wait_ge(dsem[n], self.dma_cnt[n])

            @block.tensor
            def _(eng):
                run("pe", eng)

            @block.scalar
            def _(eng):
                run("act", eng)

            @block.vector
            def _(eng):
                run("dve", eng)

            @block.gpsimd
            def _(eng):
                run("pool", eng)

            @block.sync
            def _(eng):
                run("sp", eng)


def _make_blocks():
    B = []

    def add(src, cols):
        cols = np.asarray(cols, dtype=np.int64)
        B.append({"src": src, "cols": cols, "W": len(cols)})
        return len(B) - 1

    ar = np.arange
    T = {}
    T["RQ"] = [(add("in", 0 + h * 256 + ar(128)), add("in", 0 + h * 256 + 128 + ar(128))) for h in range(4)]
    T["RK"] = [(add("in", 1024 + h * 256 + ar(128)), add("in", 1024 + h * 256 + 128 + ar(128))) for h in range(4)]
    T["RV"] = [add("in", 2048 + h * 256 + ar(256)) for h in range(4)]
    T["RG"] = [(add("in", 3072 + h * 256 + ar(128)), add("in", 3072 + h * 256 + 128 + ar(128))) for h in range(4)]

    def pairc(base, g, m, half):
        return np.concatenate([base + g * 1024 + (2 * m) * 128 + half * 64 + ar(64),
                               base + g * 1024 + (2 * m + 1) * 128 + half * 64 + ar(64)])
    T["AQ"] = [[(add("in", pairc(4096, g, m, 0)), add("in", pairc(4096, g, m, 1))) for g in range(3)] for m in range(4)]
    T["AK"] = [[(add("in", pairc(7168, g, m, 0)), add("in", pairc(7168, g, m, 1))) for g in range(3)] for m in range(4)]
    T["AV"] = [[add("in", 10240 + g * 1024 + 2 * m * 128 + ar(256)) for g in range(3)] for m in range(4)]
    T["AG"] = [add("in", 13312 + h * 128 + ar(128)) for h in range(8)]
    T["CL"] = [add("in", 14336 + c * 128 + ar(128)) for c in range(8)]
    T["CGATE"] = [add("in", 15360 + c * 128 + ar(128)) for c in range(8)]
    T["CG"] = [add("in", 16384 + c * 128 + ar(128)) for c in range(8)]
    T["MG"] = [[add("in", 17408 + b * 1024 + f * 128 + ar(128)) for f in range(8)] for b in range(3)]
    T["WO"] = [[add(("ret", "att", "conv")[b], f * 128 + ar(128)) for f in range(8)] for b in range(3)]
    T["WOUT"] = [add("out", q * 256 + ar(256)) for q in range(4)]
    off = 0
    for b in B:
        b["off"] = off
        off += 8 * b["W"]
    voff = 0
    for h in range(4):
        B[T["RV"][h]]["voff"] = voff
        voff += 256
    for m in range(4):
        for g in range(3):
            B[T["AV"][m][g]]["voff"] = voff
            voff += 256
    return B, T, off, voff


BLK, BT, WTOT, VTOT = _make_blocks()
NBLK = len(BLK)
NPV = 40 + 248
GAMMA = [1.0 - 2.0 ** (-5.0 - h) for h in range(4)]


def build_program(layers=(0, 1), nseq=NS, final=True, debug=False):
    nc = bass.Bass("TRN2", target_bir_lowering=False)
    NL = DEPTH
    x_d = nc.dram_tensor("x", [nseq, SEQ, D_MODEL], F32, kind="ExternalInput").ap()
    wt_d = nc.dram_tensor("wt", [NL, 128, WTOT], F32, kind="ExternalInput").ap()
    bt_d = nc.dram_tensor("bt", [NL, 128, NBLK], F32, kind="ExternalInput").ap()
    brow_d = nc.dram_tensor("brow", [NL, 1, VTOT], F32, kind="ExternalInput").ap()
    pv_d = nc.dram_tensor("pv", [NL, 128, NPV], F32, kind="ExternalInput").ap()
    fnw_d = nc.dram_tensor("fnw", [128, D_MODEL], F32, kind="ExternalInput").ap()
    tab_d = nc.dram_tensor("tab", [128, 4 * SEQ], F32, kind="ExternalInput").ap()
    cst_d = nc.dram_tensor("cst", [128, 520], F32, kind="ExternalInput").ap()
    cstb_d = nc.dram_tensor("cstb", [128, 384], F32, kind="ExternalInput").ap()
    skind = "ExternalOutput" if debug else "Internal"
    gs_d = nc.dram_tensor("gs", [nseq, 3, 8, 128, SEQ], BF16, kind=skind).ap()
    xres_d = nc.dram_tensor("xres", [nseq, SEQ, D_MODEL], F32, kind=skind).ap()
    out_d = nc.dram_tensor("out", [nseq, SEQ, D_MODEL], F32, kind="ExternalOutput").ap()

    es = contextlib.ExitStack()
    with es:
        ARENA_BYTES = 212000
        arena = es.enter_context(nc.sbuf_tensor("arena", [128, ARENA_BYTES // 2], BF16))
        banks = [es.enter_context(nc.psum_tensor("ps%d" % i, [128, 512], F32))[:] for i in range(8)]
        s = Sched(nc)

        class Bump:
            def __init__(self, start, end):
                self.p = start
                self.end = end

            def alloc(self, shape, dt):
                n = 1
                for d in shape[1:]:
                    n *= d
                nbytes = n * (4 if dt == F32 else 2)
                nbytes = (nbytes + 63) // 64 * 64
                off = self.p
                self.p += nbytes
                assert self.p <= self.end, ("SBUF arena overflow", self.p, self.end)
                ap = arena[:, off // 2: off // 2 + (n * (2 if dt == F32 else 1))]
                if dt == F32:
                    ap = ap.bitcast(F32)
                if len(shape) == 3:
                    ap = ap.rearrange("p (a b) -> p a b", a=shape[1])
                elif len(shape) == 4:
                    ap = ap.rearrange("p (a b c) -> p a b c", a=shape[1], b=shape[2])
                return ap[0:shape[0]] if shape[0] != 128 else ap

        pers = Bump(0, ARENA_BYTES)
        hT = pers.alloc([128, 8, SEQ], BF16)
        cst = pers.alloc([128, 520], F32)
        cstb = pers.alloc([128, 384], BF16)
        ones = pers.alloc([128, 128], BF16)
        pv = pers.alloc([128, NPV], F32)
        btile = pers.alloc([128, NBLK], F32)
        brow = pers.alloc([128, VTOT], BF16)
        fnw = pers.alloc([128, D_MODEL], F32)
        NRING = 5
        ring = [pers.alloc([128, 2048], BF16) for _ in range(NRING)]
        rtmp = [[pers.alloc([128, 512], F32) for _ in range(4)] for _ in range(2)]
        sm = pers.alloc([128, 64], F32)
        PH0 = pers.p
        decayT = cst[:, 0:512].rearrange("p (h i) -> p h i", h=4)
        qdec = cst[:, 512:516]
        kdec = cst[:, 516:520]
        maskT = cstb[:, 0:256].rearrange("p (k i) -> p k i", k=2)
        ident = cstb[:, 256:384]

        bank_i = [0]

        def bank():
            i = bank_i[0] % 6
            bank_i[0] += 1
            return banks[i], "ps%d" % i

        ring_i = [0]

        rt_i = [0]

        def mm(ps, lhsT, rhs, start, stop, reads, writes, sig=None):
            if sig is None:
                sig = stop
            s.op("pe", lambda e: e.matmul(ps, lhsT=lhsT, rhs=rhs, start=start, stop=stop), reads, writes, sig)

        def fm_group(wv, wn, rhs_fn, ps, pn, extra_reads=("hT",)):
            for kc in range(8):
                mm(ps, wv[:, kc, :], rhs_fn(kc), kc == 0, kc == 7, [wn] + list(extra_reads), [pn])

        def act(out, in_, func, reads, writes, bias=None, scale=None, accum=None):
            kw = {}
            if bias is not None:
                kw["bias"] = bias
            if scale is not None:
                kw["scale"] = scale
            if accum is not None:
                kw["accum_out"] = accum
            s.op("act", lambda e: e.activation(out=out, in_=in_, func=func, **kw), reads, writes)

        def dve(fn, reads, writes):
            s.op("dve", fn, reads, writes)

        def tt(out, in0, in1, op, reads, writes):
            dve(lambda e: e.tensor_tensor(out=out, in0=in0, in1=in1, op=op), reads, writes)

        def stt(out, in0, scalar, in1, op0, op1, reads, writes):
            dve(lambda e: e.scalar_tensor_tensor(out=out, in0=in0, scalar=scalar, in1=in1, op0=op0, op1=op1),
                reads, writes)

        def ts(out, in0, s1, s2, op0, op1, reads, writes):
            if s2 is None:
                dve(lambda e: e.tensor_scalar(out=out, in0=in0, scalar1=s1, scalar2=None, op0=op0), reads, writes)
            else:
                dve(lambda e: e.tensor_scalar(out=out, in0=in0, scalar1=s1, scalar2=s2, op0=op0, op1=op1),
                    reads, writes)

        def rstd_from(out_ap, in_ap, scale, eps, name):
            act(out_ap, in_ap, AF.Ln, [name], [name], bias=eps, scale=scale)
            act(out_ap, out_ap, AF.Exp, [name], [name], scale=-0.5)

        s.dma("sp", lambda e: e.dma_start(out=cst, in_=cst_d), "c1", writes=["cst"])
        s.dma("pool", lambda e: e.dma_start(out=cstb, in_=cstb_d), "c2", writes=["cstb"])
        s.dma("sp", lambda e: e.dma_start(out=fnw, in_=fnw_d), "c3", writes=["fnw"])
        s.op("dve", lambda e: e.memset(ones, 1.0), writes=["ones"])
        s.barrier()

        def tokslice(t4):
            return slice(t4 * 512, (t4 + 1) * 512)


        def run_pass(l, sq, src, dst_res, is_last):
            bias = lambda blk: btile[:, blk:blk + 1]

            class WQ:
                def __init__(self, blocks, la=3):
                    self.blocks = list(blocks)
                    self.h = [None] * len(self.blocks)
                    self.issued = 0
                    self.i = 0
                    self.la = la

                def _issue(self, j):
                    blk = self.blocks[j]
                    slot = ring_i[0] % NRING
                    ring_i[0] += 1
                    W = BLK[blk]["W"]
                    off = BLK[blk]["off"]
                    dstv = ring[slot][:, 0:8 * W]
                    s.dma("pool", lambda e: e.dma_start(out=dstv, in_=wt_d[l, :, off:off + 8 * W]), "w%d" % slot,
                          writes=["ring%d" % slot])
                    self.h[j] = (dstv.rearrange("p (k c) -> p k c", k=8), "ring%d" % slot)

                def next(self, blk):
                    assert self.blocks[self.i] == blk, (self.i, self.blocks[self.i], blk)
                    while self.issued < min(len(self.blocks), self.i + 1 + self.la):
                        self._issue(self.issued)
                        self.issued += 1
                    r = self.h[self.i]
                    self.i += 1
                    return r

            ph = Bump(PH0, ARENA_BYTES)
            XT = [ph.alloc([128, D_MODEL], F32) for _ in range(2)]
            XS = [ph.alloc([128, D_MODEL], BF16) for _ in range(2)]
            junk = ph.alloc([128, D_MODEL], BF16)
            nw = pv[:, 0:8]
            for n in range(16):
                b2 = n % 2
                xt, xs = XT[b2], XS[b2]
                s.dma("sp", lambda e, xt=xt, n=n: e.dma_start(out=xt, in_=src[n * 128:(n + 1) * 128, :]),
                      "xt%d" % b2, writes=["XT%d" % b2])
                ss = sm[:, b2:b2 + 1]
                dve(lambda e, ss=ss: e.memset(ss, 0.0), [], ["ss%d" % b2])
                act(junk, xt, AF.Square, ["XT%d" % b2], ["junk", "ss%d" % b2], accum=ss)
                rstd_from(ss, ss, 1.0 / D_MODEL, 1e-6, "ss%d" % b2)
                ts(xs, xt, ss, None, ALU.mult, None, ["XT%d" % b2, "ss%d" % b2], ["XS%d" % b2])
                for half in range(2):
                    ps, pn = bank()
                    for kq in range(4):
                        kc = half * 4 + kq
                        mm(ps[:, kq * 128:(kq + 1) * 128], xs[:, kc * 128:(kc + 1) * 128], ident, True, True,
                           ["XS%d" % b2, "cstb"], [pn], sig=(kq == 3))
                    tt(hT[:, half * 4:half * 4 + 4, n * 128:(n + 1) * 128],
                       ps.rearrange("p (a b) -> p a b", a=4),
                       nw[:, half * 4:half * 4 + 4].unsqueeze(2).to_broadcast([128, 4, 128]), ALU.mult,
                       [pn, "pv"], ["hT"])
            s.barrier()

            def rope_items(wq, blkA, blkB, cosT, sinT, dst_fn, dname):
                hold = {}

                def item(t4):
                    if t4 == 0:
                        hold["A"] = wq.next(blkA)
                        hold["B"] = wq.next(blkB)
                    wA, nA = hold["A"]
                    wB, nB = hold["B"]
                    tk = tokslice(t4)
                    psA, pA = bank()
                    fm_group(wA, nA, lambda kc: hT[:, kc, tk], psA, pA)
                    psB, pB = bank()
                    fm_group(wB, nB, lambda kc: hT[:, kc, tk], psB, pB)
                    rt_i[0] += 1
                    k2 = rt_i[0] % 2
                    t1, t2, t3, t4b = rtmp[k2]
                    rn = "rt%d" % k2
                    stt(t1, psA, bias(blkA), cosT[:, tk], ALU.add, ALU.mult, [pA, "bt", "tab"], [rn + "a"])
                    stt(t2, psB, bias(blkB), sinT[:, tk], ALU.add, ALU.mult, [pB, "bt", "tab"], [rn + "b"])
                    stt(t3, psB, bias(blkB), cosT[:, tk], ALU.add, ALU.mult, [pB, "bt", "tab"], [rn + "c"])
                    stt(t4b, psA, bias(blkA), sinT[:, tk], ALU.add, ALU.mult, [pA, "bt", "tab"], [rn + "d"])
                    oA, vw = dst_fn(0, t4)
                    s.op("pool", lambda e: e.tensor_tensor(out=oA, in0=vw(t1), in1=vw(t2), op=ALU.subtract),
                         [rn + "a", rn + "b"], [dname])
                    oB, vw2 = dst_fn(1, t4)
                    s.op("pool", lambda e: e.tensor_tensor(out=oB, in0=vw2(t3), in1=vw2(t4b), op=ALU.add),
                         [rn + "c", rn + "d"], [dname])
                return [lambda t4=t4: item(t4) for t4 in range(4)]

            def silu_items(wq, blk, dst_fn, dname):
                hold = {}

                def item(t4):
                    if t4 == 0:
                        hold["w"] = wq.next(blk)
                    wg, wn = hold["w"]
                    tk = tokslice(t4)
                    ps, pn = bank()
                    fm_group(wg, wn, lambda kc: hT[:, kc, tk], ps, pn)
                    act(dst_fn(tk), ps, AF.Silu, [pn, "bt"], [dname], bias=bias(blk))
                return [lambda t4=t4: item(t4) for t4 in range(4)]

            def v_items(wq, blk, dst, dname, tok_fn, per=2):
                hold = {}
                vo = BLK[blk]["voff"]

                def item(i0):
                    if i0 == 0:
                        hold["w"] = wq.next(blk)
                    wv, wn = hold["w"]
                    for n in range(i0, i0 + per):
                        ps, pn = bank()
                        for kc in range(8):
                            mm(ps[:, 0:256], tok_fn(kc, n), wv[:, kc, :], kc == 0, False, [wn, "hT"], [pn], sig=False)
                        mm(ps[:, 0:256], ones[0:1, :], brow[0:1, vo:vo + 256], False, True, ["ones", "brow"], [pn])
                        act(dst[:, n, :], ps[:, 0:256], AF.Copy, [pn], [dname])
                return [lambda i0=i0: item(i0) for i0 in range(0, 16, per)]

            ph = Bump(PH0, ARENA_BYTES)
            tab = ph.alloc([128, 4, SEQ], F32)
            cosR, sinR, cosA, sinA = tab[:, 0, :], tab[:, 1, :], tab[:, 2, :], tab[:, 3, :]
            s.dma("sp", lambda e: e.dma_start(out=tab.rearrange("p a b -> p (a b)"), in_=tab_d), "c0", writes=["tab"])
            qT = [ph.alloc([128, 2, SEQ], BF16) for _ in range(2)]
            kT = [ph.alloc([128, 2, SEQ], BF16) for _ in range(2)]
            vR = [ph.alloc([128, 16, 256], BF16) for _ in range(2)]
            rgT = [ph.alloc([128, 2, SEQ], BF16) for _ in range(2)]
            GT = ph.alloc([128, 2, SEQ], BF16)
            kS = [ph.alloc([128, 256], BF16) for _ in range(2)]
            scT = [ph.alloc([128, 128], BF16) for _ in range(2)]
            r1 = [ph.alloc([128, 256], F32) for _ in range(2)]
            rr = [ph.alloc([128, 256], F32) for _ in range(2)]
            rnb = [ph.alloc([128, 256], BF16) for _ in range(2)]
            Sf = ph.alloc([128, 512], F32)
            Sb = [ph.alloc([128, 2, 256], BF16) for _ in range(2)]
            junkr = ph.alloc([128, 256], BF16)
            rnw = pv[:, 8:16]
            rblocks = []
            for hh in range(4):
                rblocks += [BT["RQ"][hh][0], BT["RQ"][hh][1], BT["RK"][hh][0], BT["RK"][hh][1], BT["RV"][hh],
                            BT["RG"][hh][0], BT["RG"][hh][1]]
            wq = WQ(rblocks)

            def head_items(hh):
                bb = hh % 2
                nat = lambda dst: (lambda ab, t4: (dst[:, ab, tokslice(t4)], (lambda a: a)))
                it = []
                it += rope_items(wq, BT["RQ"][hh][0], BT["RQ"][hh][1], cosR, sinR, nat(qT[bb]), "qT%d" % bb)
                it += rope_items(wq, BT["RK"][hh][0], BT["RK"][hh][1], cosR, sinR, nat(kT[bb]), "kT%d" % bb)
                it += v_items(wq, BT["RV"][hh], vR[bb], "vR%d" % bb, lambda kc, n: hT[:, kc, n * 128:(n + 1) * 128])
                for ec in range(2):
                    it += silu_items(wq, BT["RG"][hh][ec], (lambda tk, ec=ec, bb=bb: rgT[bb][:, ec, tk]), "rgT%d" % bb)
                return it

            def chunk(hh, n, state):
                bb = hh % 2
                qTn, kTn, vRn, rgn = "qT%d" % bb, "kT%d" % bb, "vR%d" % bb, "rgT%d" % bb
                q_, k_, v_, g_ = qT[bb], kT[bb], vR[bb], rgT[bb]
                gC = GAMMA[hh] ** 128
                ch = slice(n * 128, (n + 1) * 128)
                b2 = n % 2
                if n < 15:
                    psT, pT = bank()
                    for dc in range(2):
                        mm(psT[:, dc * 128:(dc + 1) * 128], k_[:, dc, ch], ident, True, True, [kTn, "cstb"], [pT], sig=(dc == 1))
                    act(kS[b2], psT[:, 0:256], AF.Copy, [pT, "cst"], ["kS%d" % b2], scale=kdec[:, hh:hh + 1])
                psS, pS = bank()
                for dc in range(2):
                    mm(psS[:, 0:128], k_[:, dc, ch], q_[:, dc, ch], dc == 0, dc == 1, [kTn, qTn], [pS])
                tt(scT[b2], psS[:, 0:128], decayT[:, hh, :], ALU.mult, [pS, "cst"], ["scT%d" % b2])
                if state.get("tr") is not None:
                    state["tr"]()
                    state["tr"] = None
                if n < 15:
                    psD, pD = bank()
                    for dc in range(2):
                        mm(psD[:, dc * 256:(dc + 1) * 256], kS[b2][:, dc * 128:(dc + 1) * 128], v_[:, n, :], True, True,
                           ["kS%d" % b2, vRn], [pD], sig=(dc == 1))
                psO, pO = bank()
                mm(psO[:, 0:256], scT[b2], v_[:, n, :], True, True, ["scT%d" % b2, vRn], [pO])
                if n > 0:
                    for dc in range(2):
                        mm(psO[:, 256:512], q_[:, dc, ch], Sb[b2][:, dc, :], dc == 0, dc == 1, [qTn, "Sb%d" % b2], [pO])
                if n < 15:
                    if n == 0:
                        dve(lambda e: e.tensor_copy(out=Sf, in_=psD), [pD], ["Sf"])
                    else:
                        stt(Sf, Sf, gC, psD, ALU.mult, ALU.add, [pD, "Sf"], ["Sf"])
                    act(Sb[1 - b2].rearrange("p a b -> p (a b)"), Sf, AF.Copy, ["Sf"], ["Sb%d" % (1 - b2)])
                act(r1[b2], psO[:, 0:256], AF.Copy, [pO], ["r1%d" % b2])
                if n > 0:
                    stt(rr[b2], psO[:, 256:512], qdec[:, hh:hh + 1], r1[b2], ALU.mult, ALU.add,
                        [pO, "cst", "r1%d" % b2], ["rr%d" % b2])
                    rcur, rname = rr[b2], "rr%d" % b2
                else:
                    rcur, rname = r1[b2], "r1%d" % b2
                st = sm[:, 8 + b2 * 8: 16 + b2 * 8]
                stn = "st%d" % b2
                dve(lambda e: e.memset(st[:, 0:2], 0.0), [], [stn])
                act(junkr, rcur, AF.Square, [rname], ["junkr", stn], accum=st[:, 0:1])
                act(junkr, rcur, AF.Copy, [rname], ["junkr", stn], accum=st[:, 1:2])
                ts(st[:, 2:3], st[:, 1:2], 1.0 / 256, None, ALU.mult, None, [stn], [stn])
                tt(st[:, 3:4], st[:, 2:3], st[:, 2:3], ALU.mult, [stn], [stn])
                stt(st[:, 4:5], st[:, 0:1], 1.0 / 256, st[:, 3:4], ALU.mult, ALU.subtract, [stn], [stn])
                rstd_from(st[:, 4:5], st[:, 4:5], 1.0, 1e-5, stn)
                ts(rnb[b2], rcur, st[:, 2:3], st[:, 4:5], ALU.subtract, ALU.mult, [rname, stn], ["rnb%d" % b2])

                def do_tr():
                    psR, pR = bank()
                    for ec in range(2):
                        mm(psR[:, ec * 128:(ec + 1) * 128], rnb[b2][:, ec * 128:(ec + 1) * 128], ident, True, True,
                           ["rnb%d" % b2, "cstb"], [pR], sig=(ec == 1))
                    for ec in range(2):
                        stt(GT[:, ec, ch], psR[:, ec * 128:(ec + 1) * 128], rnw[:, 2 * hh + ec:2 * hh + ec + 1],
                            g_[:, ec, ch], ALU.mult, ALU.mult, [pR, "pv", rgn], ["GT"])
                state["tr"] = do_tr

            for f in head_items(0):
                f()
            for hh in range(4):
                nxt = head_items(hh + 1) if hh < 3 else []
                state = {}
                ni = 0
                for n in range(16):
                    chunk(hh, n, state)
                    want = (len(nxt) * (n + 1) + 15) // 16
                    while ni < want:
                        nxt[ni]()
                        ni += 1
                state["tr"]()
                s.dma("sp", lambda e, hh=hh: e.dma_start(out=gs_d[sq, 0, 2 * hh:2 * hh + 2].rearrange("c p t -> p c t"),
                                                          in_=GT), "gst0", reads=["GT"], writes=["gs0"])
            s.barrier()

            ph = Bump(PH0, ARENA_BYTES)
            tab2 = ph.alloc([128, 4, SEQ], F32)
            qA = ph.alloc([128, 2, SEQ], BF16)
            kA = ph.alloc([128, 2, SEQ], BF16)
            vA = ph.alloc([128, 16, 256], BF16)
            agT = ph.alloc([128, 2, SEQ], BF16)
            acc = [ph.alloc([128, 2, SEQ], F32) for _ in range(2)]
            EX = [ph.alloc([128, 2, 128], BF16) for _ in range(3)]
            PT = [ph.alloc([128, 2, 128], BF16) for _ in range(3)]
            GTa = ph.alloc([128, 2, SEQ], BF16)
            SCALE = 128.0 ** -0.5

            def tokset(ap2, g, jt):
                if g == 0:
                    return ap2[:, jt * 128:(jt + 1) * 128]
                if g == 1:
                    n, r = jt // 4, jt % 4
                    return ap2[:, n * 512:(n + 1) * 512].rearrange("p (l r) -> p r l", r=4)[:, r, :]
                return ap2.rearrange("p (l r) -> p r l", r=16)[:, jt, :]

            def prev_tile(g, qt):
                if g == 0:
                    return qt - 1 if qt > 0 else None
                if g == 1:
                    return qt - 4 if qt >= 4 else None
                return None

            ablocks = []
            for m in range(4):
                ablocks += [BT["AG"][2 * m], BT["AG"][2 * m + 1]]
                for g in range(3):
                    ablocks += [BT["AQ"][m][g][0], BT["AQ"][m][g][1], BT["AK"][m][g][0], BT["AK"][m][g][1], BT["AV"][m][g]]
            wq = WQ(ablocks)
            for m in range(4):
                for hp in range(2):
                    for f in silu_items(wq, BT["AG"][2 * m + hp], (lambda tk, hp=hp: agT[:, hp, tk]), "agT"):
                        f()
                for g in range(3):
                    def gdst(dst, g=g):
                        def f(ab, t4):
                            if g == 0:
                                return dst[:, ab, tokslice(t4)], (lambda a: a)
                            if g == 1:
                                return (dst[:, ab, tokslice(t4)].rearrange("p (r l) -> p l r", r=4),
                                        lambda a: a.rearrange("p (l r) -> p l r", r=4))
                            return (dst[:, ab, :].rearrange("p (r l) -> p l r", r=16)[:, t4 * 32:(t4 + 1) * 32, :],
                                    lambda a: a.rearrange("p (l r) -> p l r", r=16))
                        return f
                    for f in rope_items(wq, BT["AQ"][m][g][0], BT["AQ"][m][g][1], cosA, sinA, gdst(qA), "qA"):
                        f()
                    for f in rope_items(wq, BT["AK"][m][g][0], BT["AK"][m][g][1], cosA, sinA, gdst(kA), "kA"):
                        f()
                    for f in v_items(wq, BT["AV"][m][g], vA, "vA", lambda kc, jt, g=g: tokset(hT[:, kc, :], g, jt)):
                        f()
                    items = [(hp, qt) for hp in range(2) for qt in range(16)]

                    def stage1(i, g=g):
                        hp, qt = items[i]
                        pv_ = prev_tile(g, qt)
                        kts = ([pv_] if pv_ is not None else []) + [qt]
                        psS, pS = bank()
                        pr = slice(hp * 64, (hp + 1) * 64)
                        for kb, kt_ in enumerate(kts):
                            for ab in range(2):
                                mm(psS[:, kb * 128:(kb + 1) * 128], kA[pr, ab, kt_ * 128:(kt_ + 1) * 128],
                                   qA[pr, ab, qt * 128:(qt + 1) * 128], ab == 0, ab == 1, ["kA", "qA"], [pS],
                                   sig=(ab == 1 and kb == len(kts) - 1))
                        nk = len(kts)
                        b3 = i % 3
                        ex = EX[b3].rearrange("p a b -> p (a b)")[:, 0:nk * 128]
                        act(ex, psS[:, 0:nk * 128], AF.Exp, [pS], ["EX%d" % b3], scale=SCALE)
                        mk = maskT if nk == 2 else maskT[:, 1:2, :]
                        s.op("pool", lambda e: e.tensor_tensor(out=PT[b3][:, 0:nk, :], in0=EX[b3][:, 0:nk, :], in1=mk,
                                                               op=ALU.mult), ["EX%d" % b3, "cstb"], ["PT%d" % b3])
                        return kts

                    def stage2(i, kts, g=g):
                        hp, qt = items[i]
                        b3 = i % 3
                        psU, pU = bank()
                        nk = len(kts)
                        for kb, kt_ in enumerate(kts):
                            mm(psU[:, 0:128], vA[:, kt_, hp * 128:(hp + 1) * 128], PT[b3][:, kb, :], kb == 0, kb == nk - 1,
                               ["vA", "PT%d" % b3], [pU], sig=False)
                        for kb, kt_ in enumerate(kts):
                            mm(psU[:, 128:256], ones, PT[b3][:, kb, :], kb == 0, kb == nk - 1, ["ones", "PT%d" % b3], [pU],
                               sig=(kb == nk - 1))
                        if g == 0:
                            av = acc[hp][:, :, qt * 128:(qt + 1) * 128]
                        elif g == 1:
                            n, r = qt // 4, qt % 4
                            av = acc[hp][:, :, n * 512:(n + 1) * 512].rearrange("p u (l r) -> p u r l", r=4)[:, :, r, :]
                        else:
                            av = acc[hp].rearrange("p u (l r) -> p u r l", r=16)[:, :, qt, :]
                        pu = psU[:, 0:256].rearrange("p (u i) -> p u i", u=2)
                        if g == 0:
                            act(av, pu, AF.Copy, [pU], ["acc%d" % hp])
                        else:
                            tt(av, pu, av, ALU.add, [pU, "acc%d" % hp], ["acc%d" % hp])

                    kq = stage1(0)
                    kq1 = stage1(1)
                    for i in range(len(items)):
                        kn = stage1(i + 2) if i + 2 < len(items) else None
                        stage2(i, kq)
                        kq, kq1 = kq1, kn
                for hp in range(2):
                    an = "acc%d" % hp
                    dve(lambda e, hp=hp: e.reciprocal(out=acc[hp][:, 1, :], in_=acc[hp][:, 1, :]), [an], [an])
                    s.op("pool", lambda e, hp=hp: e.tensor_tensor(out=acc[hp][:, 0, :], in0=acc[hp][:, 0, :], in1=acc[hp][:, 1, :],
                                                                 op=ALU.mult), [an], [an])
                    s.op("pool", lambda e, hp=hp: e.tensor_tensor(out=GTa[:, hp, :], in0=acc[hp][:, 0, :], in1=agT[:, hp, :],
                                                                 op=ALU.mult), [an, "agT"], ["GTa"])
                s.dma("sp", lambda e, m=m: e.dma_start(out=gs_d[sq, 1, 2 * m:2 * m + 2].rearrange("c p t -> p c t"),
                                                        in_=GTa), "gst1", reads=["GTa"], writes=["gs1"])
            s.barrier()

            ph = Bump(PH0, ARENA_BYTES)
            cT = ph.alloc([128, 8, 544], BF16)
            scg2 = [ph.alloc([128, 8, 512], BF16) for _ in range(2)]
            cv = [ph.alloc([128, 8, 512], F32) for _ in range(2)]
            cvb = [ph.alloc([128, 512], BF16) for _ in range(2)]
            sqb = [ph.alloc([128, 512], BF16) for _ in range(2)]
            sg = [ph.alloc([128, 512], F32) for _ in range(2)]
            mean = ph.alloc([128, 512], F32)
            msq = ph.alloc([128, 512], F32)
            rstd = ph.alloc([128, 512], F32)
            xn = [ph.alloc([128, 512], F32) for _ in range(2)]
            yb = [ph.alloc([128, 512], BF16) for _ in range(2)]
            GTc = ph.alloc([128, 8, 512], BF16)
            cTs = [ph.alloc([128, 8, 544], BF16) for _ in range(1)]
            Dm2 = [ph.alloc([128, 31, 128], BF16) for _ in range(2)]
            tmpc = [ph.alloc([128, 512], F32) for _ in range(4)]
            tc_i = [0]
            dwb = pv[:, 16:24]
            lnw = pv[:, 24:32]
            lnb = pv[:, 32:40]
            dw = pv[:, 40:288].rearrange("p (c t) -> p c t", c=8)
            cTb = [cT, cTs[0]]
            dve(lambda e: e.memset(cTb[0][:, :, 0:30], 0.0), [], ["cT0"])
            cblocks = []
            for t4 in range(4):
                for cc in range(8):
                    cblocks += [BT["CL"][cc], BT["CGATE"][cc], BT["CG"][cc]]
            wq = WQ(cblocks)
            psSum, pSum = banks[6], "ps6"
            psSq, pSq = banks[7], "ps7"

            def conv_proj(t4):
                tk = tokslice(t4)
                c_, cn = cTb[t4 % 2], "cT%d" % (t4 % 2)
                scg = scg2[t4 % 2]
                if t4 > 0:
                    pvb = cTb[(t4 - 1) % 2]
                    dve(lambda e: e.tensor_copy(out=c_[:, :, 0:30], in_=pvb[:, :, 512:542]), ["cT%d" % ((t4 - 1) % 2)], [cn])
                for cc in range(8):
                    wl, nl = wq.next(BT["CL"][cc])
                    psL, pL = bank()
                    fm_group(wl, nl, lambda kc: hT[:, kc, tk], psL, pL)
                    wg, ng = wq.next(BT["CGATE"][cc])
                    psG, pG = bank()
                    fm_group(wg, ng, lambda kc: hT[:, kc, tk], psG, pG)
                    wc, ncg = wq.next(BT["CG"][cc])
                    psC, pC = bank()
                    fm_group(wc, ncg, lambda kc: hT[:, kc, tk], psC, pC)
                    b2 = cc % 2
                    act(sg[b2], psG, AF.Sigmoid, [pG, "bt"], ["sg%d" % b2], bias=bias(BT["CGATE"][cc]))
                    stt(c_[:, cc, 30:542], psL, bias(BT["CL"][cc]), sg[b2], ALU.add, ALU.mult,
                        [pL, "bt", "sg%d" % b2], [cn])
                    act(scg[:, cc, :], psC, AF.Silu, [pC, "bt"], ["scg%d" % (t4 % 2)], bias=bias(BT["CG"][cc]))

            def conv_taps(t4):
                c_, cn = cTb[t4 % 2], "cT%d" % (t4 % 2)
                cv_ = cv[t4 % 2]
                for cc in range(8):
                    d2 = cc % 2
                    Dm = Dm2[d2]
                    s.op("pool", lambda e, Dm=Dm, cc=cc: e.tensor_tensor(out=Dm, in0=ident.unsqueeze(1).to_broadcast([128, 31, 128]),
                         in1=dw[:, cc, :].unsqueeze(2).to_broadcast([128, 31, 128]), op=ALU.mult), ["cstb", "pv"], ["Dm%d" % d2])
                    psV, pV = bank()
                    for tau in range(31):
                        mm(psV, Dm[:, tau, :], c_[:, cc, tau:tau + 512], tau == 0, tau == 30, ["Dm%d" % d2, cn], [pV])
                    nm = "cv%d_%d" % (t4 % 2, cc)
                    act(cv_[:, cc, :], psV, AF.Identity, [pV, "pv"], [nm], bias=dwb[:, cc:cc + 1])

            def conv_norm(t4):
                tk = tokslice(t4)
                cv_ = cv[t4 % 2]
                scg = scg2[t4 % 2]
                for cc in range(8):
                    b2 = cc % 2
                    nm = "cv%d_%d" % (t4 % 2, cc)
                    act(sqb[b2], cv_[:, cc, :], AF.Square, [nm], ["sqb%d" % b2])
                    act(cvb[b2], cv_[:, cc, :], AF.Copy, [nm], ["cvb%d" % b2])
                    mm(psSum, ones, cvb[b2], cc == 0, cc == 7, ["ones", "cvb%d" % b2], [pSum], sig=True)
                    mm(psSq, ones, sqb[b2], cc == 0, cc == 7, ["ones", "sqb%d" % b2], [pSq], sig=True)
                act(mean, psSum, AF.Copy, [pSum], ["mean"], scale=1.0 / 1024)
                tt(msq, mean, mean, ALU.mult, ["mean"], ["msq"])
                stt(rstd, psSq, 1.0 / 1024, msq, ALU.mult, ALU.subtract, [pSq, "msq"], ["rstd"])
                rstd_from(rstd, rstd, 1.0, 1e-5, "rstd")
                for cc in range(8):
                    b2 = cc % 2
                    nm = "cv%d_%d" % (t4 % 2, cc)
                    tt(xn[b2], cv_[:, cc, :], mean, ALU.subtract, [nm, "mean"], ["xn%d" % b2])
                    tt(xn[b2], xn[b2], rstd, ALU.mult, ["xn%d" % b2, "rstd"], ["xn%d" % b2])
                    act(yb[b2], xn[b2], AF.Silu, ["xn%d" % b2, "pv"], ["yb%d" % b2], bias=lnb[:, cc:cc + 1],
                        scale=lnw[:, cc:cc + 1])
                    tt(GTc[:, cc, :], yb[b2], scg[:, cc, :], ALU.mult, ["yb%d" % b2, "scg%d" % (t4 % 2)], ["GTc"])
                s.dma("sp", lambda e: e.dma_start(out=gs_d[sq, 2, :, :, tk].rearrange("c p t -> p c t"), in_=GTc),
                      "gst2", reads=["GTc"], writes=["gs2"])

            if CONV_PIPE:
                conv_proj(0)
                for t4 in range(4):
                    if t4 < 3:
                        conv_proj(t4 + 1)
                    conv_taps(t4)
                    conv_norm(t4)
            else:
                for t4 in range(4):
                    conv_proj(t4)
                    conv_taps(t4)
                    conv_norm(t4)
            s.barrier()

            ph = Bump(PH0, ARENA_BYTES)
            MT = 1024
            Gb = [ph.alloc([128, 8, MT], BF16) for _ in range(3)]
            mTb = ph.alloc([128, 8, MT], BF16)
            WOb = [ph.alloc([128, 8, 256], BF16) for _ in range(4)]
            sgm = [ph.alloc([128, 512], F32) for _ in range(3)]
            tm = [ph.alloc([128, 512], F32) for _ in range(6)]
            xtm = [ph.alloc([128, D_MODEL], F32) for _ in range(2)]
            xo = [ph.alloc([128, D_MODEL], F32) for _ in range(2)]
            junkm = ph.alloc([128, D_MODEL], BF16)
            for q in range(4):
                blk = BT["WOUT"][q]
                off = BLK[blk]["off"]
                s.dma("pool", lambda e, q=q, off=off: e.dma_start(out=WOb[q].rearrange("p a b -> p (a b)"),
                                                                  in_=wt_d[l, :, off:off + 2048]),
                      "wo%d" % q, writes=["WOb%d" % q])
            mblocks = []
            for t2 in range(SEQ // MT):
                for fc in range(8):
                    for b in range(3):
                        mblocks += [BT["WO"][b][fc], BT["MG"][b][fc]]
            wq = WQ(mblocks)
            for t2 in range(SEQ // MT):
                tk2 = slice(t2 * MT, (t2 + 1) * MT)
                for b in range(3):
                    s.dma("sp", lambda e, b=b, tk2=tk2: e.dma_start(out=Gb[b], in_=gs_d[sq, b, :, :, tk2].rearrange("c p t -> p c t")),
                          "gb%d" % b, reads=["gs%d" % b], writes=["Gb%d" % b])
                NT5 = MT // 512
                for fc in range(8):
                    for b in range(3):
                        wy, ny = wq.next(BT["WO"][b][fc])
                        wg, ng = wq.next(BT["MG"][b][fc])
                        for t5 in range(NT5):
                            tl = slice(t5 * 512, (t5 + 1) * 512)
                            tg = slice(t2 * MT + t5 * 512, t2 * MT + (t5 + 1) * 512)
                            j = b * NT5 + t5
                            psY, pY = bank()
                            fm_group(wy, ny, lambda kc, b=b, tl=tl: Gb[b][:, kc, tl], psY, pY, extra_reads=("Gb%d" % b,))
                            psG, pG = bank()
                            fm_group(wg, ng, lambda kc, tg=tg: hT[:, kc, tg], psG, pG)
                            j3 = j % 3
                            act(sgm[j3], psG, AF.Sigmoid, [pG, "bt"], ["sgm%d" % j3], bias=bias(BT["MG"][b][fc]))
                            tt(tm[j], psY, sgm[j3], ALU.mult, [pY, "sgm%d" % j3], ["tm%d" % j])
                    for t5 in range(NT5):
                        tl = slice(t5 * 512, (t5 + 1) * 512)
                        j0, j1, j2 = t5, NT5 + t5, 2 * NT5 + t5
                        s.op("pool", lambda e, j0=j0, j1=j1: e.tensor_tensor(out=tm[j0], in0=tm[j0], in1=tm[j1], op=ALU.add),
                             ["tm%d" % j0, "tm%d" % j1], ["tm%d" % j0])
                        s.op("pool", lambda e, fc=fc, tl=tl, j0=j0, j2=j2: e.tensor_tensor(out=mTb[:, fc, tl], in0=tm[j0], in1=tm[j2],
                                                                                      op=ALU.add),
                             ["tm%d" % j0, "tm%d" % j2], ["mTb"])
                for ti in range(MT // 128):
                    tok0 = t2 * MT + ti * 128
                    b2 = ti % 2
                    s.dma("sp", lambda e, b2=b2, tok0=tok0: e.dma_start(out=xtm[b2], in_=src[tok0:tok0 + 128, :]),
                          "xm%d" % b2, writes=["xtm%d" % b2])
                    for half in range(2):
                        psO, pO = bank()
                        for qq in range(2):
                            q = half * 2 + qq
                            for kc in range(8):
                                mm(psO[:, qq * 256:(qq + 1) * 256], mTb[:, kc, ti * 128:(ti + 1) * 128], WOb[q][:, kc, :],
                                   kc == 0, kc == 7, ["mTb", "WOb%d" % q], [pO], sig=(kc == 7 and qq == 1))
                        tt(xo[b2][:, half * 512:(half + 1) * 512], psO, xtm[b2][:, half * 512:(half + 1) * 512], ALU.add,
                           [pO, "xtm%d" % b2], ["xo%d" % b2])
                    if is_last:
                        ss = sm[:, 32 + b2:33 + b2]
                        ssn = "fs%d" % b2
                        dve(lambda e, ss=ss: e.memset(ss, 0.0), [], [ssn])
                        act(junkm, xo[b2], AF.Square, ["xo%d" % b2], ["junkm", ssn], accum=ss)
                        rstd_from(ss, ss, 1.0 / D_MODEL, 1e-6, ssn)
                        stt(xo[b2], xo[b2], ss, fnw, ALU.mult, ALU.mult, ["xo%d" % b2, ssn, "fnw"], ["xo%d" % b2])
                        s.dma("sp", lambda e, b2=b2, tok0=tok0: e.dma_start(out=out_d[sq, tok0:tok0 + 128, :], in_=xo[b2]),
                              "xs%d" % b2, reads=["xo%d" % b2], writes=["outd"])
                    else:
                        s.dma("sp", lambda e, b2=b2, tok0=tok0: e.dma_start(out=dst_res[tok0:tok0 + 128, :], in_=xo[b2]),
                              "xs%d" % b2, reads=["xo%d" % b2], writes=["xres%d" % sq])
            s.barrier()

        for li, l in enumerate(layers):
            s.dma("sp", lambda e, l=l: e.dma_start(out=pv, in_=pv_d[l]), "c4", writes=["pv"])
            s.dma("sp", lambda e, l=l: e.dma_start(out=btile, in_=bt_d[l]), "c5", writes=["bt"])
            s.dma("pool", lambda e, l=l: e.dma_start(out=brow[0:1, :], in_=brow_d[l]), "c6", writes=["brow"])
            s.barrier()
            for sq in range(nseq):
                src = x_d[sq] if li == 0 else xres_d[sq]
                last = (li == len(layers) - 1) and final
                run_pass(l, sq, src, xres_d[sq] if li < len(layers) - 1 or not final else None, last)
        fw = ["xs0", "xs1"]
        s.emit(final_waits=fw)
    return nc


def _host_tables():
    t = np.arange(SEQ, dtype=np.float32)

    def cs(hd, rows):
        inv = (np.float32(THETA) ** (-np.arange(0, hd, 2, dtype=np.float32) / np.float32(hd))).astype(np.float32)
        ang = (t[:, None] * inv[None, :]).astype(np.float32)
        c = np.cos(ang).astype(np.float32).T
        sn = np.sin(ang).astype(np.float32).T
        idx = np.arange(128) % rows
        return c[idx], sn[idx]
    cR, sR = cs(256, 128)
    cA, sA = cs(128, 64)
    tab = np.concatenate([cR, sR, cA, sA], axis=1).astype(np.float32)
    i = np.arange(128)
    cst = np.zeros((128, 520), np.float32)
    for h in range(4):
        lg = np.log(np.float32(GAMMA[h])).astype(np.float32)
        diff = (i[None, :] - i[:, None]).astype(np.float32)
        dec = np.where(diff >= 0, np.exp(np.maximum(diff, 0) * lg), 0.0) / 16.0
        cst[:, h * 128:(h + 1) * 128] = dec
        cst[:, 512 + h] = np.exp((i + 1.0) * lg)
        cst[:, 516 + h] = np.exp((127.0 - i) * lg) / 16.0
    cstb = np.zeros((128, 384), np.float32)
    cstb[:, 0:128] = (i[None, :] <= i[:, None])
    cstb[:, 128:256] = (i[None, :] >= i[:, None])
    cstb[:, 256:384] = np.eye(128)
    return tab, cst, cstb


def _host_pack(inp):
    srcs = {"in": inp["w_in"], "ret": inp["ret_w_o"], "att": inp["att_w_o"], "conv": inp["conv_w_o"], "out": inp["w_out"]}
    wt = np.empty((DEPTH, 128, WTOT), np.float32)
    bt = np.zeros((DEPTH, 128, NBLK), np.float32)
    brow = np.zeros((DEPTH, 1, VTOT), np.float32)
    pv = np.zeros((DEPTH, 128, NPV), np.float32)
    for l in range(DEPTH):
        for bi, b in enumerate(BLK):
            W = b["W"]
            blkw = srcs[b["src"]][l][:, b["cols"]]
            wt[l, :, b["off"]:b["off"] + 8 * W] = blkw.reshape(8, 128, W).transpose(1, 0, 2).reshape(128, 8 * W)
            if b["src"] == "in":
                if W == 128:
                    bt[l, :, bi] = inp["b_in"][l][b["cols"]]
                else:
                    brow[l, 0, b["voff"]:b["voff"] + W] = inp["b_in"][l][b["cols"]]
        f8 = lambda v: v.reshape(8, 128).T
        pv[l, :, 0:8] = f8(inp["norm_w"][l])
        pv[l, :, 8:16] = f8(inp["ret_norm_w"][l])
        pv[l, :, 16:24] = f8(inp["conv_dw_b"][l])
        pv[l, :, 24:32] = f8(inp["conv_norm_w"][l])
        pv[l, :, 32:40] = f8(inp["conv_norm_b"][l])
        pv[l, :, 40:288] = inp["conv_dw_w"][l].reshape(31, 8, 128).transpose(2, 1, 0).reshape(128, 248)
    fnw = np.ascontiguousarray(np.broadcast_to(inp["final_norm_w"][None, :], (128, D_MODEL))).astype(np.float32)
    return wt, bt, brow, pv, fnw


_CACHE = {}


def kernel(**inputs):
    inp = {k: np.asarray(v, dtype=np.float32) for k, v in inputs.items()}
    x = inp["x"]
    wt, bt, brow, pv, fnw = _host_pack(inp)
    tab, cst, cstb = _host_tables()
    if "nc" not in _CACHE:
        _CACHE["nc"] = build_program()
    nc = _CACHE["nc"]
    in_maps = []
    for c in range(NCORES):
        in_maps.append({"x": np.ascontiguousarray(x[c * NS:(c + 1) * NS]), "wt": wt, "bt": bt, "brow": brow, "pv": pv,
                        "fnw": fnw, "tab": tab, "cst": cst, "cstb": cstb})
    res = run_bass_kernel_spmd(nc, in_maps, core_ids=list(range(NCORES)))
    out = np.concatenate([np.asarray(r["out"], dtype=np.float32) for r in res.results], axis=0)
    return out
```

```python
import contextlib
import numpy as np
import concourse.bass as bass
import concourse.mybir as mybir
from concourse.bass_utils import run_bass_kernel_spmd

F32 = mybir.dt.float32
BF16 = mybir.dt.bfloat16
AF = mybir.ActivationFunctionType
ALU = mybir.AluOpType

ENGS = ("pe", "act", "dve", "pool", "sp")

D_MODEL = 1024
SEQ = 2048
DEPTH = 2
NCORES = 8
CONV_POOL = (0, 1, 2, 3, 4)
CONV_PIPE = True
NS = 2
THETA = 10000.0


class Sched:
    def __init__(self, nc):
        self.nc = nc
        self.streams = {e: [] for e in ENGS}
        self.res = {}
        self.dma_cnt = {}
        self.dma_names = []
        self.pending = {e: [] for e in ENGS}

    def _deps_for(self, eng, reads, writes):
        deps = []
        if self.pending[eng]:
            deps.extend(self.pending[eng])
            self.pending[eng] = []
        for r in reads:
            st = self.res.get(r)
            if st and st["w"] is not None:
                deps.append(st["w"])
        for w in writes:
            st = self.res.get(w)
            if st:
                if st["w"] is not None:
                    deps.append(st["w"])
                deps.extend(st["r"])
        return deps

    def _commit(self, token, reads, writes):
        for r in reads:
            st = self.res.setdefault(r, {"w": None, "r": []})
            st["r"].append(token)
            if len(st["r"]) > 16:
                seen = {}
                for t in st["r"]:
                    k = t[:2]
                    if k not in seen or seen[k][2] < t[2]:
                        seen[k] = t
                st["r"] = list(seen.values())
        for w in writes:
            self.res[w] = {"w": token, "r": []}

    def op(self, eng, fn, reads=(), writes=(), sig=True):
        deps = self._deps_for(eng, reads, writes)
        idx = len(self.streams[eng])
        self.streams[eng].append({"fn": fn, "deps": deps, "sig": sig, "dma": None})
        self._commit(("eng", eng, idx), reads, writes)

    def dma(self, q, fn, sem, reads=(), writes=()):
        deps = self._deps_for(q, reads, writes)
        if sem not in self.dma_cnt:
            self.dma_cnt[sem] = 0
            self.dma_names.append(sem)
        self.dma_cnt[sem] += 16
        self.streams[q].append({"fn": fn, "deps": deps, "sig": False, "dma": sem})
        self._commit(("dma", sem, self.dma_cnt[sem]), reads, writes)

    def barrier(self):
        toks = []
        for e in ENGS:
            st = self.streams[e]
            for i in range(len(st) - 1, -1, -1):
                if st[i]["dma"] is None:
                    toks.append(("eng", e, i))
                    break
        for n in self.dma_names:
            toks.append(("dma", n, self.dma_cnt[n]))
        for e in ENGS:
            self.pending[e] = list(toks)

    def emit(self, final_waits=()):
        nc = self.nc
        sigcnt = {}
        for e in ENGS:
            st = self.streams[e]
            for o in reversed(st):
                if o["dma"] is None:
                    o["sig"] = True
                    break
            c = 0
            arr = []
            for o in st:
                if o["dma"] is None and o["sig"]:
                    c += 1
                arr.append(c)
            need = [None] * len(st)
            nxt = None
            for i in range(len(st) - 1, -1, -1):
                if st[i]["dma"] is None and st[i]["sig"]:
                    nxt = arr[i]
                need[i] = nxt
            sigcnt[e] = need
        with contextlib.ExitStack() as es:
            esem = {e: es.enter_context(nc.semaphore("s_" + e)) for e in ENGS}
            dsem = {n: es.enter_context(nc.semaphore("d_" + n)) for n in self.dma_names}
            block = es.enter_context(nc.Block())

            def run(e, eng):
                waited = {}
                for o in self.streams[e]:
                    wl = {}
                    for d in o["deps"]:
                        if d[0] == "eng":
                            if d[1] == e and e == "pe":
                                continue
                            key = ("eng", d[1])
                            val = sigcnt[d[1]][d[2]]
                        else:
                            key = ("dma", d[1])
                            val = d[2]
                        if val is None or waited.get(key, 0) >= val:
                            continue
                        if wl.get(key, 0) < val:
                            wl[key] = val
                    for key, val in wl.items():
                        sem = esem[key[1]] if key[0] == "eng" else dsem[key[1]]
                        eng.wait_ge(sem, val)
                        waited[key] = val
                    ins = o["fn"](eng)
                    if o["dma"] is not None:
                        ins.then_inc(dsem[o["dma"]], 16)
                    elif o["sig"]:
                        ins.then_inc(esem[e], 1)
                if e == "sp":
                    for n in final_waits:
                        eng.wait_ge(dsem[n], self.dma_cnt[n])

            @block.tensor
            def _(eng):
                run("pe", eng)

            @block.scalar
            def _(eng):
                run("act", eng)

            @block.vector
            def _(eng):
                run("dve", eng)

            @block.gpsimd
            def _(eng):
                run("pool", eng)

            @block.sync
            def _(eng):
                run("sp", eng)


def _make_blocks():
    B = []

    def add(src, cols):
        cols = np.asarray(cols, dtype=np.int64)
        B.append({"src": src, "cols": cols, "W": len(cols)})
        return len(B) - 1

    ar = np.arange
    T = {}
    T["RQ"] = [(add("in", 0 + h * 256 + ar(128)), add("in", 0 + h * 256 + 128 + ar(128))) for h in range(4)]
    T["RK"] = [(add("in", 1024 + h * 256 + ar(128)), add("in", 1024 + h * 256 + 128 + ar(128))) for h in range(4)]
    T["RV"] = [add("in", 2048 + h * 256 + ar(256)) for h in range(4)]
    T["RG"] = [(add("in", 3072 + h * 256 + ar(128)), add("in", 3072 + h * 256 + 128 + ar(128))) for h in range(4)]

    def pairc(base, g, m, half):
        return np.concatenate([base + g * 1024 + (2 * m) * 128 + half * 64 + ar(64),
                               base + g * 1024 + (2 * m + 1) * 128 + half * 64 + ar(64)])
    T["AQ"] = [[(add("in", pairc(4096, g, m, 0)), add("in", pairc(4096, g, m, 1))) for g in range(3)] for m in range(4)]
    T["AK"] = [[(add("in", pairc(7168, g, m, 0)), add("in", pairc(7168, g, m, 1))) for g in range(3)] for m in range(4)]
    T["AV"] = [[add("in", 10240 + g * 1024 + 2 * m * 128 + ar(256)) for g in range(3)] for m in range(4)]
    T["AG"] = [add("in", 13312 + h * 128 + ar(128)) for h in range(8)]
    T["CL"] = [add("in", 14336 + c * 128 + ar(128)) for c in range(8)]
    T["CGATE"] = [add("in", 15360 + c * 128 + ar(128)) for c in range(8)]
    T["CG"] = [add("in", 16384 + c * 128 + ar(128)) for c in range(8)]
    T["MG"] = [[add("in", 17408 + b * 1024 + f * 128 + ar(128)) for f in range(8)] for b in range(3)]
    T["WO"] = [[add(("ret", "att", "conv")[b], f * 128 + ar(128)) for f in range(8)] for b in range(3)]
    T["WOUT"] = [add("out", q * 256 + ar(256)) for q in range(4)]
    off = 0
    for b in B:
        b["off"] = off
        off += 8 * b["W"]
    voff = 0
    for h in range(4):
        B[T["RV"][h]]["voff"] = voff
        voff += 256
    for m in range(4):
        for g in range(3):
            B[T["AV"][m][g]]["voff"] = voff
            voff += 256
    return B, T, off, voff


BLK, BT, WTOT, VTOT = _make_blocks()
NBLK = len(BLK)
NPV = 40 + 248
GAMMA = [1.0 - 2.0 ** (-5.0 - h) for h in range(4)]


def build_program(layers=(0, 1), nseq=NS, final=True, debug=False):
    nc = bass.Bass("TRN2", target_bir_lowering=False)
    NL = DEPTH
    x_d = nc.dram_tensor("x", [nseq, SEQ, D_MODEL], F32, kind="ExternalInput").ap()
    wt_d = nc.dram_tensor("wt", [NL, 128, WTOT], F32, kind="ExternalInput").ap()
    bt_d = nc.dram_tensor("bt", [NL, 128, NBLK], F32, kind="ExternalInput").ap()
    brow_d = nc.dram_tensor("brow", [NL, 1, VTOT], F32, kind="ExternalInput").ap()
    pv_d = nc.dram_tensor("pv", [NL, 128, NPV], F32, kind="ExternalInput").ap()
    fnw_d = nc.dram_tensor("fnw", [128, D_MODEL], F32, kind="ExternalInput").ap()
    tab_d = nc.dram_tensor("tab", [128, 4 * SEQ], F32, kind="ExternalInput").ap()
    cst_d = nc.dram_tensor("cst", [128, 520], F32, kind="ExternalInput").ap()
    cstb_d = nc.dram_tensor("cstb", [128, 384], F32, kind="ExternalInput").ap()
    skind = "ExternalOutput" if debug else "Internal"
    gs_d = nc.dram_tensor("gs", [nseq, 3, 8, 128, SEQ], BF16, kind=skind).ap()
    xres_d = nc.dram_tensor("xres", [nseq, SEQ, D_MODEL], F32, kind=skind).ap()
    out_d = nc.dram_tensor("out", [nseq, SEQ, D_MODEL], F32, kind="ExternalOutput").ap()

    es = contextlib.ExitStack()
    with es:
        ARENA_BYTES = 212000
        arena = es.enter_context(nc.sbuf_tensor("arena", [128, ARENA_BYTES // 2], BF16))
        banks = [es.enter_context(nc.psum_tensor("ps%d" % i, [128, 512], F32))[:] for i in range(8)]
        s = Sched(nc)

        class Bump:
            def __init__(self, start, end):
                self.p = start
                self.end = end

            def alloc(self, shape, dt):
                n = 1
                for d in shape[1:]:
                    n *= d
                nbytes = n * (4 if dt == F32 else 2)
                nbytes = (nbytes + 63) // 64 * 64
                off = self.p
                self.p += nbytes
                assert self.p <= self.end, ("SBUF arena overflow", self.p, self.end)
                ap = arena[:, off // 2: off // 2 + (n * (2 if dt == F32 else 1))]
                if dt == F32:
                    ap = ap.bitcast(F32)
                if len(shape) == 3:
                    ap = ap.rearrange("p (a b) -> p a b", a=shape[1])
                elif len(shape) == 4:
                    ap = ap.rearrange("p (a b c) -> p a b c", a=shape[1], b=shape[2])
                return ap[0:shape[0]] if shape[0] != 128 else ap

        pers = Bump(0, ARENA_BYTES)
        hT = pers.alloc([128, 8, SEQ], BF16)
        cst = pers.alloc([128, 520], F32)
        cstb = pers.alloc([128, 384], BF16)
        ones = pers.alloc([128, 128], BF16)
        pv = pers.alloc([128, NPV], F32)
        btile = pers.alloc([128, NBLK], F32)
        brow = pers.alloc([128, VTOT], BF16)
        fnw = pers.alloc([128, D_MODEL], F32)
        NRING = 5
        ring = [pers.alloc([128, 2048], BF16) for _ in range(NRING)]
        rtmp = [[pers.alloc([128, 512], F32) for _ in range(4)] for _ in range(2)]
        sm = pers.alloc([128, 64], F32)
        PH0 = pers.p
        decayT = cst[:, 0:512].rearrange("p (h i) -> p h i", h=4)
        qdec = cst[:, 512:516]
        kdec = cst[:, 516:520]
        maskT = cstb[:, 0:256].rearrange("p (k i) -> p k i", k=2)
        ident = cstb[:, 256:384]

        bank_i = [0]

        def bank():
            i = bank_i[0] % 6
            bank_i[0] += 1
            return banks[i], "ps%d" % i

        ring_i = [0]

        rt_i = [0]

        def mm(ps, lhsT, rhs, start, stop, reads, writes, sig=None):
            if sig is None:
                sig = stop
            s.op("pe", lambda e: e.matmul(ps, lhsT=lhsT, rhs=rhs, start=start, stop=stop), reads, writes, sig)

        def fm_group(wv, wn, rhs_fn, ps, pn, extra_reads=("hT",)):
            for kc in range(8):
                mm(ps, wv[:, kc, :], rhs_fn(kc), kc == 0, kc == 7, [wn] + list(extra_reads), [pn])

        def act(out, in_, func, reads, writes, bias=None, scale=None, accum=None):
            kw = {}
            if bias is not None:
                kw["bias"] = bias
            if scale is not None:
                kw["scale"] = scale
            if accum is not None:
                kw["accum_out"] = accum
            s.op("act", lambda e: e.activation(out=out, in_=in_, func=func, **kw), reads, writes)

        def dve(fn, reads, writes):
            s.op("dve", fn, reads, writes)

        def tt(out, in0, in1, op, reads, writes):
            dve(lambda e: e.tensor_tensor(out=out, in0=in0, in1=in1, op=op), reads, writes)

        def stt(out, in0, scalar, in1, op0, op1, reads, writes):
            dve(lambda e: e.scalar_tensor_tensor(out=out, in0=in0, scalar=scalar, in1=in1, op0=op0, op1=op1),
                reads, writes)

        def ts(out, in0, s1, s2, op0, op1, reads, writes):
            if s2 is None:
                dve(lambda e: e.tensor_scalar(out=out, in0=in0, scalar1=s1, scalar2=None, op0=op0), reads, writes)
            else:
                dve(lambda e: e.tensor_scalar(out=out, in0=in0, scalar1=s1, scalar2=s2, op0=op0, op1=op1),
                    reads, writes)

        def rstd_from(out_ap, in_ap, scale, eps, name):
            act(out_ap, in_ap, AF.Sqrt, [name], [name], bias=eps, scale=scale)
            dve(lambda e: e.reciprocal(out=out_ap, in_=out_ap), [name], [name])

        s.dma("sp", lambda e: e.dma_start(out=cst, in_=cst_d), "c1", writes=["cst"])
        s.dma("pool", lambda e: e.dma_start(out=cstb, in_=cstb_d), "c2", writes=["cstb"])
        s.dma("sp", lambda e: e.dma_start(out=fnw, in_=fnw_d), "c3", writes=["fnw"])
        s.op("dve", lambda e: e.memset(ones, 1.0), writes=["ones"])
        s.barrier()

        def tokslice(t4):
            return slice(t4 * 512, (t4 + 1) * 512)


        def run_pass(l, sq, src, dst_res, is_last):
            bias = lambda blk: btile[:, blk:blk + 1]

            class WQ:
                def __init__(self, blocks, la=3):
                    self.blocks = list(blocks)
                    self.h = [None] * len(self.blocks)
                    self.issued = 0
                    self.i = 0
                    self.la = la

                def _issue(self, j):
                    blk = self.blocks[j]
                    slot = ring_i[0] % NRING
                    ring_i[0] += 1
                    W = BLK[blk]["W"]
                    off = BLK[blk]["off"]
                    dstv = ring[slot][:, 0:8 * W]
                    s.dma("pool", lambda e: e.dma_start(out=dstv, in_=wt_d[l, :, off:off + 8 * W]), "w%d" % slot,
                          writes=["ring%d" % slot])
                    self.h[j] = (dstv.rearrange("p (k c) -> p k c", k=8), "ring%d" % slot)

                def next(self, blk):
                    assert self.blocks[self.i] == blk, (self.i, self.blocks[self.i], blk)
                    while self.issued < min(len(self.blocks), self.i + 1 + self.la):
                        self._issue(self.issued)
                        self.issued += 1
                    r = self.h[self.i]
                    self.i += 1
                    return r

            ph = Bump(PH0, ARENA_BYTES)
            XT = [ph.alloc([128, D_MODEL], F32) for _ in range(2)]
            XS = [ph.alloc([128, D_MODEL], BF16) for _ in range(2)]
            junk = ph.alloc([128, D_MODEL], BF16)
            nw = pv[:, 0:8]
            for n in range(16):
                b2 = n % 2
                xt, xs = XT[b2], XS[b2]
                s.dma("sp", lambda e, xt=xt, n=n: e.dma_start(out=xt, in_=src[n * 128:(n + 1) * 128, :]),
                      "xt%d" % b2, writes=["XT%d" % b2])
                ss = sm[:, b2:b2 + 1]
                dve(lambda e, ss=ss: e.memset(ss, 0.0), [], ["ss%d" % b2])
                act(junk, xt, AF.Square, ["XT%d" % b2], ["junk", "ss%d" % b2], accum=ss)
                rstd_from(ss, ss, 1.0 / D_MODEL, 1e-6, "ss%d" % b2)
                ts(xs, xt, ss, None, ALU.mult, None, ["XT%d" % b2, "ss%d" % b2], ["XS%d" % b2])
                for half in range(2):
                    ps, pn = bank()
                    for kq in range(4):
                        kc = half * 4 + kq
                        mm(ps[:, kq * 128:(kq + 1) * 128], xs[:, kc * 128:(kc + 1) * 128], ident, True, True,
                           ["XS%d" % b2, "cstb"], [pn], sig=(kq == 3))
                    tt(hT[:, half * 4:half * 4 + 4, n * 128:(n + 1) * 128],
                       ps.rearrange("p (a b) -> p a b", a=4),
                       nw[:, half * 4:half * 4 + 4].unsqueeze(2).to_broadcast([128, 4, 128]), ALU.mult,
                       [pn, "pv"], ["hT"])
            s.barrier()

            def rope_items(wq, blkA, blkB, cosT, sinT, dst_fn, dname):
                hold = {}

                def item(t4):
                    if t4 == 0:
                        hold["A"] = wq.next(blkA)
                        hold["B"] = wq.next(blkB)
                    wA, nA = hold["A"]
                    wB, nB = hold["B"]
                    tk = tokslice(t4)
                    psA, pA = bank()
                    fm_group(wA, nA, lambda kc: hT[:, kc, tk], psA, pA)
                    psB, pB = bank()
                    fm_group(wB, nB, lambda kc: hT[:, kc, tk], psB, pB)
                    rt_i[0] += 1
                    k2 = rt_i[0] % 2
                    t1, t2, t3, t4b = rtmp[k2]
                    rn = "rt%d" % k2
                    stt(t1, psA, bias(blkA), cosT[:, tk], ALU.add, ALU.mult, [pA, "bt", "tab"], [rn + "a"])
                    stt(t2, psB, bias(blkB), sinT[:, tk], ALU.add, ALU.mult, [pB, "bt", "tab"], [rn + "b"])
                    stt(t3, psB, bias(blkB), cosT[:, tk], ALU.add, ALU.mult, [pB, "bt", "tab"], [rn + "c"])
                    stt(t4b, psA, bias(blkA), sinT[:, tk], ALU.add, ALU.mult, [pA, "bt", "tab"], [rn + "d"])
                    oA, vw = dst_fn(0, t4)
                    s.op("pool", lambda e: e.tensor_tensor(out=oA, in0=vw(t1), in1=vw(t2), op=ALU.subtract),
                         [rn + "a", rn + "b"], [dname])
                    oB, vw2 = dst_fn(1, t4)
                    s.op("pool", lambda e: e.tensor_tensor(out=oB, in0=vw2(t3), in1=vw2(t4b), op=ALU.add),
                         [rn + "c", rn + "d"], [dname])
                return [lambda t4=t4: item(t4) for t4 in range(4)]

            def silu_items(wq, blk, dst_fn, dname):
                hold = {}

                def item(t4):
                    if t4 == 0:
                        hold["w"] = wq.next(blk)
                    wg, wn = hold["w"]
                    tk = tokslice(t4)
                    ps, pn = bank()
                    fm_group(wg, wn, lambda kc: hT[:, kc, tk], ps, pn)
                    act(dst_fn(tk), ps, AF.Silu, [pn, "bt"], [dname], bias=bias(blk))
                return [lambda t4=t4: item(t4) for t4 in range(4)]

            def v_items(wq, blk, dst, dname, tok_fn, per=2):
                hold = {}
                vo = BLK[blk]["voff"]

                def item(i0):
                    if i0 == 0:
                        hold["w"] = wq.next(blk)
                    wv, wn = hold["w"]
                    for n in range(i0, i0 + per):
                        ps, pn = bank()
                        for kc in range(8):
                            mm(ps[:, 0:256], tok_fn(kc, n), wv[:, kc, :], kc == 0, False, [wn, "hT"], [pn], sig=False)
                        mm(ps[:, 0:256], ones[0:1, :], brow[0:1, vo:vo + 256], False, True, ["ones", "brow"], [pn])
                        act(dst[:, n, :], ps[:, 0:256], AF.Copy, [pn], [dname])
                return [lambda i0=i0: item(i0) for i0 in range(0, 16, per)]

            ph = Bump(PH0, ARENA_BYTES)
            tab = ph.alloc([128, 4, SEQ], F32)
            cosR, sinR, cosA, sinA = tab[:, 0, :], tab[:, 1, :], tab[:, 2, :], tab[:, 3, :]
            s.dma("sp", lambda e: e.dma_start(out=tab.rearrange("p a b -> p (a b)"), in_=tab_d), "c0", writes=["tab"])
            qT = [ph.alloc([128, 2, SEQ], BF16) for _ in range(2)]
            kT = [ph.alloc([128, 2, SEQ], BF16) for _ in range(2)]
            vR = [ph.alloc([128, 16, 256], BF16) for _ in range(2)]
            rgT = [ph.alloc([128, 2, SEQ], BF16) for _ in range(2)]
            GT = ph.alloc([128, 2, SEQ], BF16)
            kS = [ph.alloc([128, 256], BF16) for _ in range(2)]
            scT = [ph.alloc([128, 128], BF16) for _ in range(2)]
            r1 = [ph.alloc([128, 256], F32) for _ in range(2)]
            rr = [ph.alloc([128, 256], F32) for _ in range(2)]
            rnb = [ph.alloc([128, 256], BF16) for _ in range(2)]
            Sf = ph.alloc([128, 512], F32)
            Sb = [ph.alloc([128, 2, 256], BF16) for _ in range(2)]
            junkr = ph.alloc([128, 256], BF16)
            rnw = pv[:, 8:16]
            rblocks = []
            for hh in range(4):
                rblocks += [BT["RQ"][hh][0], BT["RQ"][hh][1], BT["RK"][hh][0], BT["RK"][hh][1], BT["RV"][hh],
                            BT["RG"][hh][0], BT["RG"][hh][1]]
            wq = WQ(rblocks)

            def head_items(hh):
                bb = hh % 2
                nat = lambda dst: (lambda ab, t4: (dst[:, ab, tokslice(t4)], (lambda a: a)))
                it = []
                it += rope_items(wq, BT["RQ"][hh][0], BT["RQ"][hh][1], cosR, sinR, nat(qT[bb]), "qT%d" % bb)
                it += rope_items(wq, BT["RK"][hh][0], BT["RK"][hh][1], cosR, sinR, nat(kT[bb]), "kT%d" % bb)
                it += v_items(wq, BT["RV"][hh], vR[bb], "vR%d" % bb, lambda kc, n: hT[:, kc, n * 128:(n + 1) * 128])
                for ec in range(2):
                    it += silu_items(wq, BT["RG"][hh][ec], (lambda tk, ec=ec, bb=bb: rgT[bb][:, ec, tk]), "rgT%d" % bb)
                return it

            def chunk(hh, n, state):
                bb = hh % 2
                qTn, kTn, vRn, rgn = "qT%d" % bb, "kT%d" % bb, "vR%d" % bb, "rgT%d" % bb
                q_, k_, v_, g_ = qT[bb], kT[bb], vR[bb], rgT[bb]
                gC = GAMMA[hh] ** 128
                ch = slice(n * 128, (n + 1) * 128)
                b2 = n % 2
                if n < 15:
                    psT, pT = bank()
                    for dc in range(2):
                        mm(psT[:, dc * 128:(dc + 1) * 128], k_[:, dc, ch], ident, True, True, [kTn, "cstb"], [pT], sig=(dc == 1))
                    ts(kS[b2], psT[:, 0:256], kdec[:, hh:hh + 1], None, ALU.mult, None, [pT, "cst"], ["kS%d" % b2])
                psS, pS = bank()
                for dc in range(2):
                    mm(psS[:, 0:128], k_[:, dc, ch], q_[:, dc, ch], dc == 0, dc == 1, [kTn, qTn], [pS])
                tt(scT[b2], psS[:, 0:128], decayT[:, hh, :], ALU.mult, [pS, "cst"], ["scT%d" % b2])
                if n < 15:
                    psD, pD = bank()
                    for dc in range(2):
                        mm(psD[:, dc * 256:(dc + 1) * 256], kS[b2][:, dc * 128:(dc + 1) * 128], v_[:, n, :], True, True,
                           ["kS%d" % b2, vRn], [pD], sig=(dc == 1))
                psO, pO = banks[6 + b2], "ps%d" % (6 + b2)
                mm(psO[:, 0:256], scT[b2], v_[:, n, :], True, True, ["scT%d" % b2, vRn], [pO])
                if n > 0:
                    for dc in range(2):
                        mm(psO[:, 256:512], q_[:, dc, ch], Sb[b2][:, dc, :], dc == 0, dc == 1, [qTn, "Sb%d" % b2], [pO])
                if n < 15:
                    if n == 0:
                        dve(lambda e: e.tensor_copy(out=Sf, in_=psD), [pD], ["Sf"])
                    else:
                        stt(Sf, Sf, gC, psD, ALU.mult, ALU.add, [pD, "Sf"], ["Sf"])
                    dve(lambda e: e.tensor_copy(out=Sb[1 - b2].rearrange("p a b -> p (a b)"), in_=Sf), ["Sf"], ["Sb%d" % (1 - b2)])
                def do_norm():
                    act(r1[b2], psO[:, 0:256], AF.Copy, [pO], ["r1%d" % b2])
                    if n > 0:
                        stt(rr[b2], psO[:, 256:512], qdec[:, hh:hh + 1], r1[b2], ALU.mult, ALU.add,
                            [pO, "cst", "r1%d" % b2], ["rr%d" % b2])
                        rcur, rname = rr[b2], "rr%d" % b2
                    else:
                        rcur, rname = r1[b2], "r1%d" % b2
                    st = sm[:, 8 + b2 * 8: 16 + b2 * 8]
                    stn = "st%d" % b2
                    dve(lambda e: e.memset(st[:, 0:2], 0.0), [], [stn])
                    act(junkr, rcur, AF.Square, [rname], ["junkr", stn], accum=st[:, 0:1])
                    act(junkr, rcur, AF.Copy, [rname], ["junkr", stn], accum=st[:, 1:2])
                    ts(st[:, 2:3], st[:, 1:2], 1.0 / 256, None, ALU.mult, None, [stn], [stn])
                    tt(st[:, 3:4], st[:, 2:3], st[:, 2:3], ALU.mult, [stn], [stn])
                    stt(st[:, 4:5], st[:, 0:1], 1.0 / 256, st[:, 3:4], ALU.mult, ALU.subtract, [stn], [stn])
                    rstd_from(st[:, 4:5], st[:, 4:5], 1.0, 1e-5, stn)
                    ts(rnb[b2], rcur, st[:, 2:3], st[:, 4:5], ALU.subtract, ALU.mult, [rname, stn], ["rnb%d" % b2])
                    return do_tr

                def do_tr():
                    psR, pR = bank()
                    for ec in range(2):
                        mm(psR[:, ec * 128:(ec + 1) * 128], rnb[b2][:, ec * 128:(ec + 1) * 128], ident, True, True,
                           ["rnb%d" % b2, "cstb"], [pR], sig=(ec == 1))
                    for ec in range(2):
                        stt(GT[:, ec, ch], psR[:, ec * 128:(ec + 1) * 128], rnw[:, 2 * hh + ec:2 * hh + ec + 1],
                            g_[:, ec, ch], ALU.mult, ALU.mult, [pR, "pv", rgn], ["GT"])
                pn_ = state.pop("norm", None)
                pt_ = state.pop("tr", None)
                if pn_ is not None:
                    state["tr"] = pn_()
                if pt_ is not None:
                    pt_()
                state["norm"] = do_norm

            for f in head_items(0):
                f()
            for hh in range(4):
                nxt = head_items(hh + 1) if hh < 3 else []
                state = {}
                ni = 0
                for n in range(16):
                    chunk(hh, n, state)
                    want = (len(nxt) * (n + 1) + 15) // 16
                    while ni < want:
                        nxt[ni]()
                        ni += 1
                pt_ = state.pop("tr", None)
                tr_last = state.pop("norm")()
                if pt_ is not None:
                    pt_()
                tr_last()
                s.dma("sp", lambda e, hh=hh: e.dma_start(out=gs_d[sq, 0, 2 * hh:2 * hh + 2].rearrange("c p t -> p c t"),
                                                          in_=GT), "gst0", reads=["GT"], writes=["gs0"])
            s.barrier()

            ph = Bump(PH0, ARENA_BYTES)
            tab2 = ph.alloc([128, 4, SEQ], F32)
            qA = ph.alloc([128, 2, SEQ], BF16)
            kA = ph.alloc([128, 2, SEQ], BF16)
            vA = ph.alloc([128, 16, 256], BF16)
            agT = ph.alloc([128, 2, SEQ], BF16)
            acc = [ph.alloc([128, 2, SEQ], F32) for _ in range(2)]
            EX = [ph.alloc([128, 2, 128], BF16) for _ in range(3)]
            PT = [ph.alloc([128, 2, 128], BF16) for _ in range(3)]
            GTa = ph.alloc([128, 2, SEQ], BF16)
            SCALE = 128.0 ** -0.5

            def tokset(ap2, g, jt):
                if g == 0:
                    return ap2[:, jt * 128:(jt + 1) * 128]
                if g == 1:
                    n, r = jt // 4, jt % 4
                    return ap2[:, n * 512:(n + 1) * 512].rearrange("p (l r) -> p r l", r=4)[:, r, :]
                return ap2.rearrange("p (l r) -> p r l", r=16)[:, jt, :]

            def prev_tile(g, qt):
                if g == 0:
                    return qt - 1 if qt > 0 else None
                if g == 1:
                    return qt - 4 if qt >= 4 else None
                return None

            ablocks = []
            for m in range(4):
                ablocks += [BT["AG"][2 * m], BT["AG"][2 * m + 1]]
                for g in range(3):
                    ablocks += [BT["AQ"][m][g][0], BT["AQ"][m][g][1], BT["AK"][m][g][0], BT["AK"][m][g][1], BT["AV"][m][g]]
            wq = WQ(ablocks)
            for m in range(4):
                for hp in range(2):
                    for f in silu_items(wq, BT["AG"][2 * m + hp], (lambda tk, hp=hp: agT[:, hp, tk]), "agT"):
                        f()
                for g in range(3):
                    def gdst(dst, g=g):
                        def f(ab, t4):
                            if g == 0:
                                return dst[:, ab, tokslice(t4)], (lambda a: a)
                            if g == 1:
                                return (dst[:, ab, tokslice(t4)].rearrange("p (r l) -> p l r", r=4),
                                        lambda a: a.rearrange("p (l r) -> p l r", r=4))
                            return (dst[:, ab, :].rearrange("p (r l) -> p l r", r=16)[:, t4 * 32:(t4 + 1) * 32, :],
                                    lambda a: a.rearrange("p (l r) -> p l r", r=16))
                        return f
                    for f in rope_items(wq, BT["AQ"][m][g][0], BT["AQ"][m][g][1], cosA, sinA, gdst(qA), "qA"):
                        f()
                    for f in rope_items(wq, BT["AK"][m][g][0], BT["AK"][m][g][1], cosA, sinA, gdst(kA), "kA"):
                        f()
                    for f in v_items(wq, BT["AV"][m][g], vA, "vA", lambda kc, jt, g=g: tokset(hT[:, kc, :], g, jt)):
                        f()
                    items = [(hp, qt) for hp in range(2) for qt in range(16)]

                    def stage1(i, g=g):
                        hp, qt = items[i]
                        pv_ = prev_tile(g, qt)
                        kts = ([pv_] if pv_ is not None else []) + [qt]
                        psS, pS = bank()
                        pr = slice(hp * 64, (hp + 1) * 64)
                        for kb, kt_ in enumerate(kts):
                            for ab in range(2):
                                mm(psS[:, kb * 128:(kb + 1) * 128], kA[pr, ab, kt_ * 128:(kt_ + 1) * 128],
                                   qA[pr, ab, qt * 128:(qt + 1) * 128], ab == 0, ab == 1, ["kA", "qA"], [pS],
                                   sig=(ab == 1 and kb == len(kts) - 1))
                        nk = len(kts)
                        b3 = i % 3
                        ex = EX[b3].rearrange("p a b -> p (a b)")[:, 0:nk * 128]
                        act(ex, psS[:, 0:nk * 128], AF.Exp, [pS], ["EX%d" % b3], scale=SCALE)
                        mk = maskT if nk == 2 else maskT[:, 1:2, :]
                        s.op("pool", lambda e: e.tensor_tensor(out=PT[b3][:, 0:nk, :], in0=EX[b3][:, 0:nk, :], in1=mk,
                                                               op=ALU.mult), ["EX%d" % b3, "cstb"], ["PT%d" % b3])
                        return kts

                    def stage2(i, kts, g=g):
                        hp, qt = items[i]
                        b3 = i % 3
                        psU, pU = bank()
                        nk = len(kts)
                        for kb, kt_ in enumerate(kts):
                            mm(psU[:, 0:128], vA[:, kt_, hp * 128:(hp + 1) * 128], PT[b3][:, kb, :], kb == 0, kb == nk - 1,
                               ["vA", "PT%d" % b3], [pU], sig=False)
                        for kb, kt_ in enumerate(kts):
                            mm(psU[:, 128:256], ones, PT[b3][:, kb, :], kb == 0, kb == nk - 1, ["ones", "PT%d" % b3], [pU],
                               sig=(kb == nk - 1))
                        if g == 0:
                            av = acc[hp][:, :, qt * 128:(qt + 1) * 128]
                        elif g == 1:
                            n, r = qt // 4, qt % 4
                            av = acc[hp][:, :, n * 512:(n + 1) * 512].rearrange("p u (l r) -> p u r l", r=4)[:, :, r, :]
                        else:
                            av = acc[hp].rearrange("p u (l r) -> p u r l", r=16)[:, :, qt, :]
                        pu = psU[:, 0:256].rearrange("p (u i) -> p u i", u=2)
                        if g == 0:
                            act(av, pu, AF.Copy, [pU], ["acc%d" % hp])
                        else:
                            tt(av, pu, av, ALU.add, [pU, "acc%d" % hp], ["acc%d" % hp])

                    kq = stage1(0)
                    kq1 = stage1(1)
                    for i in range(len(items)):
                        kn = stage1(i + 2) if i + 2 < len(items) else None
                        stage2(i, kq)
                        kq, kq1 = kq1, kn
                for hp in range(2):
                    an = "acc%d" % hp
                    dve(lambda e, hp=hp: e.reciprocal(out=acc[hp][:, 1, :], in_=acc[hp][:, 1, :]), [an], [an])
                    s.op("pool", lambda e, hp=hp: e.tensor_tensor(out=acc[hp][:, 0, :], in0=acc[hp][:, 0, :], in1=acc[hp][:, 1, :],
                                                                 op=ALU.mult), [an], [an])
                    s.op("pool", lambda e, hp=hp: e.tensor_tensor(out=GTa[:, hp, :], in0=acc[hp][:, 0, :], in1=agT[:, hp, :],
                                                                 op=ALU.mult), [an, "agT"], ["GTa"])
                s.dma("sp", lambda e, m=m: e.dma_start(out=gs_d[sq, 1, 2 * m:2 * m + 2].rearrange("c p t -> p c t"),
                                                        in_=GTa), "gst1", reads=["GTa"], writes=["gs1"])
            s.barrier()

            ph = Bump(PH0, ARENA_BYTES)
            cT = ph.alloc([128, 8, 544], BF16)
            scg2 = [ph.alloc([128, 8, 512], BF16) for _ in range(2)]
            cv = [ph.alloc([128, 8, 512], F32) for _ in range(2)]
            cvb = [ph.alloc([128, 512], BF16) for _ in range(2)]
            sqb = [ph.alloc([128, 512], BF16) for _ in range(2)]
            sg = [ph.alloc([128, 512], F32) for _ in range(2)]
            mean = ph.alloc([128, 512], F32)
            msq = ph.alloc([128, 512], F32)
            rstd = ph.alloc([128, 512], F32)
            xn = [ph.alloc([128, 512], F32) for _ in range(2)]
            yb = [ph.alloc([128, 512], BF16) for _ in range(2)]
            GTc = ph.alloc([128, 8, 512], BF16)
            cTs = [ph.alloc([128, 8, 544], BF16) for _ in range(1)]
            Dm2 = [ph.alloc([128, 31, 128], BF16) for _ in range(2)]
            tmpc = [ph.alloc([128, 512], F32) for _ in range(4)]
            tc_i = [0]
            dwb = pv[:, 16:24]
            lnw = pv[:, 24:32]
            lnb = pv[:, 32:40]
            dw = pv[:, 40:288].rearrange("p (c t) -> p c t", c=8)
            cTb = [cT, cTs[0]]
            dve(lambda e: e.memset(cTb[0][:, :, 0:30], 0.0), [], ["cT0"])
            cblocks = []
            for t4 in range(4):
                for cc in range(8):
                    cblocks += [BT["CL"][cc], BT["CGATE"][cc], BT["CG"][cc]]
            wq = WQ(cblocks)
            psSum, pSum = banks[6], "ps6"
            psSq, pSq = banks[7], "ps7"

            def conv_proj(t4):
                tk = tokslice(t4)
                c_, cn = cTb[t4 % 2], "cT%d" % (t4 % 2)
                scg = scg2[t4 % 2]
                if t4 > 0:
                    pvb = cTb[(t4 - 1) % 2]
                    dve(lambda e: e.tensor_copy(out=c_[:, :, 0:30], in_=pvb[:, :, 512:542]), ["cT%d" % ((t4 - 1) % 2)], [cn])
                for cc in range(8):
                    wl, nl = wq.next(BT["CL"][cc])
                    psL, pL = bank()
                    fm_group(wl, nl, lambda kc: hT[:, kc, tk], psL, pL)
                    wg, ng = wq.next(BT["CGATE"][cc])
                    psG, pG = bank()
                    fm_group(wg, ng, lambda kc: hT[:, kc, tk], psG, pG)
                    wc, ncg = wq.next(BT["CG"][cc])
                    psC, pC = bank()
                    fm_group(wc, ncg, lambda kc: hT[:, kc, tk], psC, pC)
                    b2 = cc % 2
                    act(sg[b2], psG, AF.Sigmoid, [pG, "bt"], ["sg%d" % b2], bias=bias(BT["CGATE"][cc]))
                    stt(c_[:, cc, 30:542], psL, bias(BT["CL"][cc]), sg[b2], ALU.add, ALU.mult,
                        [pL, "bt", "sg%d" % b2], [cn])
                    act(scg[:, cc, :], psC, AF.Silu, [pC, "bt"], ["scg%d" % (t4 % 2)], bias=bias(BT["CG"][cc]))

            def conv_taps(t4):
                c_, cn = cTb[t4 % 2], "cT%d" % (t4 % 2)
                cv_ = cv[t4 % 2]
                for cc in range(8):
                    d2 = cc % 2
                    Dm = Dm2[d2]
                    s.op("pool", lambda e, Dm=Dm, cc=cc: e.tensor_tensor(out=Dm, in0=ident.unsqueeze(1).to_broadcast([128, 31, 128]),
                         in1=dw[:, cc, :].unsqueeze(2).to_broadcast([128, 31, 128]), op=ALU.mult), ["cstb", "pv"], ["Dm%d" % d2])
                    psV, pV = bank()
                    for tau in range(31):
                        mm(psV, Dm[:, tau, :], c_[:, cc, tau:tau + 512], tau == 0, tau == 30, ["Dm%d" % d2, cn], [pV])
                    nm = "cv%d_%d" % (t4 % 2, cc)
                    act(cv_[:, cc, :], psV, AF.Identity, [pV, "pv"], [nm], bias=dwb[:, cc:cc + 1])

            def conv_norm(t4):
                tk = tokslice(t4)
                cv_ = cv[t4 % 2]
                scg = scg2[t4 % 2]
                for cc in range(8):
                    b2 = cc % 2
                    nm = "cv%d_%d" % (t4 % 2, cc)
                    act(sqb[b2], cv_[:, cc, :], AF.Square, [nm], ["sqb%d" % b2])
                    act(cvb[b2], cv_[:, cc, :], AF.Copy, [nm], ["cvb%d" % b2])
                    mm(psSum, ones, cvb[b2], cc == 0, cc == 7, ["ones", "cvb%d" % b2], [pSum], sig=True)
                    mm(psSq, ones, sqb[b2], cc == 0, cc == 7, ["ones", "sqb%d" % b2], [pSq], sig=True)
                act(mean, psSum, AF.Copy, [pSum], ["mean"], scale=1.0 / 1024)
                tt(msq, mean, mean, ALU.mult, ["mean"], ["msq"])
                stt(rstd, psSq, 1.0 / 1024, msq, ALU.mult, ALU.subtract, [pSq, "msq"], ["rstd"])
                rstd_from(rstd, rstd, 1.0, 1e-5, "rstd")
                for cc in range(8):
                    b2 = cc % 2
                    nm = "cv%d_%d" % (t4 % 2, cc)
                    tt(xn[b2], cv_[:, cc, :], mean, ALU.subtract, [nm, "mean"], ["xn%d" % b2])
                    tt(xn[b2], xn[b2], rstd, ALU.mult, ["xn%d" % b2, "rstd"], ["xn%d" % b2])
                    act(yb[b2], xn[b2], AF.Silu, ["xn%d" % b2, "pv"], ["yb%d" % b2], bias=lnb[:, cc:cc + 1],
                        scale=lnw[:, cc:cc + 1])
                    tt(GTc[:, cc, :], yb[b2], scg[:, cc, :], ALU.mult, ["yb%d" % b2, "scg%d" % (t4 % 2)], ["GTc"])
                s.dma("sp", lambda e: e.dma_start(out=gs_d[sq, 2, :, :, tk].rearrange("c p t -> p c t"), in_=GTc),
                      "gst2", reads=["GTc"], writes=["gs2"])

            if CONV_PIPE:
                conv_proj(0)
                for t4 in range(4):
                    if t4 < 3:
                        conv_proj(t4 + 1)
                    conv_taps(t4)
                    conv_norm(t4)
            else:
                for t4 in range(4):
                    conv_proj(t4)
                    conv_taps(t4)
                    conv_norm(t4)
            s.barrier()

            ph = Bump(PH0, ARENA_BYTES)
            MT = 1024
            Gb = [ph.alloc([128, 8, MT], BF16) for _ in range(3)]
            mTb = ph.alloc([128, 8, MT], BF16)
            WOb = [ph.alloc([128, 8, 256], BF16) for _ in range(4)]
            sgm = [ph.alloc([128, 512], F32) for _ in range(3)]
            tm = [ph.alloc([128, 512], F32) for _ in range(6)]
            xtm = [ph.alloc([128, D_MODEL], F32) for _ in range(2)]
            xo = [ph.alloc([128, D_MODEL], F32) for _ in range(2)]
            junkm = ph.alloc([128, D_MODEL], BF16)
            for q in range(4):
                blk = BT["WOUT"][q]
                off = BLK[blk]["off"]
                s.dma("pool", lambda e, q=q, off=off: e.dma_start(out=WOb[q].rearrange("p a b -> p (a b)"),
                                                                  in_=wt_d[l, :, off:off + 2048]),
                      "wo%d" % q, writes=["WOb%d" % q])
            mblocks = []
            for t2 in range(SEQ // MT):
                for fc in range(8):
                    for b in range(3):
                        mblocks += [BT["WO"][b][fc], BT["MG"][b][fc]]
            wq = WQ(mblocks)
            for t2 in range(SEQ // MT):
                tk2 = slice(t2 * MT, (t2 + 1) * MT)
                for b in range(3):
                    s.dma("sp", lambda e, b=b, tk2=tk2: e.dma_start(out=Gb[b], in_=gs_d[sq, b, :, :, tk2].rearrange("c p t -> p c t")),
                          "gb%d" % b, reads=["gs%d" % b], writes=["Gb%d" % b])
                NT5 = MT // 512
                for fc in range(8):
                    for b in range(3):
                        wy, ny = wq.next(BT["WO"][b][fc])
                        wg, ng = wq.next(BT["MG"][b][fc])
                        for t5 in range(NT5):
                            tl = slice(t5 * 512, (t5 + 1) * 512)
                            tg = slice(t2 * MT + t5 * 512, t2 * MT + (t5 + 1) * 512)
                            j = b * NT5 + t5
                            psY, pY = bank()
                            fm_group(wy, ny, lambda kc, b=b, tl=tl: Gb[b][:, kc, tl], psY, pY, extra_reads=("Gb%d" % b,))
                            psG, pG = bank()
                            fm_group(wg, ng, lambda kc, tg=tg: hT[:, kc, tg], psG, pG)
                            j3 = j % 3
                            act(sgm[j3], psG, AF.Sigmoid, [pG, "bt"], ["sgm%d" % j3], bias=bias(BT["MG"][b][fc]))
                            tt(tm[j], psY, sgm[j3], ALU.mult, [pY, "sgm%d" % j3], ["tm%d" % j])
                    for t5 in range(NT5):
                        tl = slice(t5 * 512, (t5 + 1) * 512)
                        j0, j1, j2 = t5, NT5 + t5, 2 * NT5 + t5
                        s.op("pool", lambda e, j0=j0, j1=j1: e.tensor_tensor(out=tm[j0], in0=tm[j0], in1=tm[j1], op=ALU.add),
                             ["tm%d" % j0, "tm%d" % j1], ["tm%d" % j0])
                        s.op("pool", lambda e, fc=fc, tl=tl, j0=j0, j2=j2: e.tensor_tensor(out=mTb[:, fc, tl], in0=tm[j0], in1=tm[j2],
                                                                                      op=ALU.add),
                             ["tm%d" % j0, "tm%d" % j2], ["mTb"])
                for ti in range(MT // 128):
                    tok0 = t2 * MT + ti * 128
                    b2 = ti % 2
                    s.dma("sp", lambda e, b2=b2, tok0=tok0: e.dma_start(out=xtm[b2], in_=src[tok0:tok0 + 128, :]),
                          "xm%d" % b2, writes=["xtm%d" % b2])
                    for half in range(2):
                        psO, pO = bank()
                        for qq in range(2):
                            q = half * 2 + qq
                            for kc in range(8):
                                mm(psO[:, qq * 256:(qq + 1) * 256], mTb[:, kc, ti * 128:(ti + 1) * 128], WOb[q][:, kc, :],
                                   kc == 0, kc == 7, ["mTb", "WOb%d" % q], [pO], sig=(kc == 7 and qq == 1))
                        tt(xo[b2][:, half * 512:(half + 1) * 512], psO, xtm[b2][:, half * 512:(half + 1) * 512], ALU.add,
                           [pO, "xtm%d" % b2], ["xo%d" % b2])
                    if is_last:
                        ss = sm[:, 32 + b2:33 + b2]
                        ssn = "fs%d" % b2
                        dve(lambda e, ss=ss: e.memset(ss, 0.0), [], [ssn])
                        act(junkm, xo[b2], AF.Square, ["xo%d" % b2], ["junkm", ssn], accum=ss)
                        rstd_from(ss, ss, 1.0 / D_MODEL, 1e-6, ssn)
                        stt(xo[b2], xo[b2], ss, fnw, ALU.mult, ALU.mult, ["xo%d" % b2, ssn, "fnw"], ["xo%d" % b2])
                        s.dma("sp", lambda e, b2=b2, tok0=tok0: e.dma_start(out=out_d[sq, tok0:tok0 + 128, :], in_=xo[b2]),
                              "xs%d" % b2, reads=["xo%d" % b2], writes=["outd"])
                    else:
                        s.dma("sp", lambda e, b2=b2, tok0=tok0: e.dma_start(out=dst_res[tok0:tok0 + 128, :], in_=xo[b2]),
                              "xs%d" % b2, reads=["xo%d" % b2], writes=["xres%d" % sq])
            s.barrier()

        for li, l in enumerate(layers):
            s.dma("sp", lambda e, l=l: e.dma_start(out=pv, in_=pv_d[l]), "c4", writes=["pv"])
            s.dma("sp", lambda e, l=l: e.dma_start(out=btile, in_=bt_d[l]), "c5", writes=["bt"])
            s.dma("pool", lambda e, l=l: e.dma_start(out=brow[0:1, :], in_=brow_d[l]), "c6", writes=["brow"])
            s.barrier()
            for sq in range(nseq):
                src = x_d[sq] if li == 0 else xres_d[sq]
                last = (li == len(layers) - 1) and final
                run_pass(l, sq, src, xres_d[sq] if li < len(layers) - 1 or not final else None, last)
        fw = ["xs0", "xs1"]
        s.emit(final_waits=fw)
    return nc


def _host_tables():
    t = np.arange(SEQ, dtype=np.float32)

    def cs(hd, rows):
        inv = (np.float32(THETA) ** (-np.arange(0, hd, 2, dtype=np.float32) / np.float32(hd))).astype(np.float32)
        ang = (t[:, None] * inv[None, :]).astype(np.float32)
        c = np.cos(ang).astype(np.float32).T
        sn = np.sin(ang).astype(np.float32).T
        idx = np.arange(128) % rows
        return c[idx], sn[idx]
    cR, sR = cs(256, 128)
    cA, sA = cs(128, 64)
    tab = np.concatenate([cR, sR, cA, sA], axis=1).astype(np.float32)
    i = np.arange(128)
    cst = np.zeros((128, 520), np.float32)
    for h in range(4):
        lg = np.log(np.float32(GAMMA[h])).astype(np.float32)
        diff = (i[None, :] - i[:, None]).astype(np.float32)
        dec = np.where(diff >= 0, np.exp(np.maximum(diff, 0) * lg), 0.0) / 16.0
        cst[:, h * 128:(h + 1) * 128] = dec
        cst[:, 512 + h] = np.exp((i + 1.0) * lg)
        cst[:, 516 + h] = np.exp((127.0 - i) * lg) / 16.0
    cstb = np.zeros((128, 384), np.float32)
    cstb[:, 0:128] = (i[None, :] <= i[:, None])
    cstb[:, 128:256] = (i[None, :] >= i[:, None])
    cstb[:, 256:384] = np.eye(128)
    return tab, cst, cstb


def _host_pack(inp):
    srcs = {"in": inp["w_in"], "ret": inp["ret_w_o"], "att": inp["att_w_o"], "conv": inp["conv_w_o"], "out": inp["w_out"]}
    wt = np.empty((DEPTH, 128, WTOT), np.float32)
    bt = np.zeros((DEPTH, 128, NBLK), np.float32)
    brow = np.zeros((DEPTH, 1, VTOT), np.float32)
    pv = np.zeros((DEPTH, 128, NPV), np.float32)
    for l in range(DEPTH):
        for bi, b in enumerate(BLK):
            W = b["W"]
            blkw = srcs[b["src"]][l][:, b["cols"]]
            wt[l, :, b["off"]:b["off"] + 8 * W] = blkw.reshape(8, 128, W).transpose(1, 0, 2).reshape(128, 8 * W)
            if b["src"] == "in":
                if W == 128:
                    bt[l, :, bi] = inp["b_in"][l][b["cols"]]
                else:
                    brow[l, 0, b["voff"]:b["voff"] + W] = inp["b_in"][l][b["cols"]]
        f8 = lambda v: v.reshape(8, 128).T
        pv[l, :, 0:8] = f8(inp["norm_w"][l])
        pv[l, :, 8:16] = f8(inp["ret_norm_w"][l])
        pv[l, :, 16:24] = f8(inp["conv_dw_b"][l])
        pv[l, :, 24:32] = f8(inp["conv_norm_w"][l])
        pv[l, :, 32:40] = f8(inp["conv_norm_b"][l])
        pv[l, :, 40:288] = inp["conv_dw_w"][l].reshape(31, 8, 128).transpose(2, 1, 0).reshape(128, 248)
    fnw = np.ascontiguousarray(np.broadcast_to(inp["final_norm_w"][None, :], (128, D_MODEL))).astype(np.float32)
    return wt, bt, brow, pv, fnw


_CACHE = {}


def kernel(**inputs):
    inp = {k: np.asarray(v, dtype=np.float32) for k, v in inputs.items()}
    x = inp["x"]
    wt, bt, brow, pv, fnw = _host_pack(inp)
    tab, cst, cstb = _host_tables()
    if "nc" not in _CACHE:
        _CACHE["nc"] = build_program()
    nc = _CACHE["nc"]
    in_maps = []
    for c in range(NCORES):
        in_maps.append({"x": np.ascontiguousarray(x[c * NS:(c + 1) * NS]), "wt": wt, "bt": bt, "brow": brow, "pv": pv,
                        "fnw": fnw, "tab": tab, "cst": cst, "cstb": cstb})
    res = run_bass_kernel_spmd(nc, in_maps, core_ids=list(range(NCORES)))
    out = np.concatenate([np.asarray(r["out"], dtype=np.float32) for r in res.results], axis=0)
    return out
```
